# Optimizing a Trainium2 kernel written in Bass

```python
import jax, jax.numpy as jnp
from jax import lax
import numpy as np

D_MODEL = 1024
BATCH = 8
SEQ = 4096
DEPTH = 1

CTX_LEN = 256
GRID_W = 64
ROPE_THETA = 10000.0
NORM_EPS = 1e-6
N_MOD = 6
NEG_INF = -1e30

MLA_HEADS = 8
MLA_NOPE = 64
MLA_ROPE = 32
MLA_QK = MLA_NOPE + MLA_ROPE
MLA_V = 64
MLA_Q_RANK = 384
MLA_KV_RANK = 256
Q_BLOCK = 128

SWA_HEADS = 8
SWA_KV_HEADS = 2
SWA_GROUP = SWA_HEADS // SWA_KV_HEADS
SWA_HD = 64
WINDOW = 128
BAND_BLOCK = 128

N_EXPERTS = 32
TOP_K = 4
D_EXPERT = D_MODEL
SWIGLU_LIMIT = 7.0
SWIGLU_ALPHA = 1.702
MOE_BLOCK = 256

KV_COLS = MLA_KV_RANK + MLA_ROPE + 2 * SWA_KV_HEADS * SWA_HD
KV_OFFSETS = (MLA_KV_RANK, MLA_KV_RANK + MLA_ROPE, MLA_KV_RANK + MLA_ROPE + SWA_KV_HEADS * SWA_HD)
Q_OFFSETS = (MLA_Q_RANK, MLA_Q_RANK + SWA_HEADS * SWA_HD, MLA_Q_RANK + SWA_HEADS * SWA_HD + D_MODEL)
IN_COLS = KV_COLS + MLA_Q_RANK + SWA_HEADS * SWA_HD + 2 * D_MODEL

kernel_name = "hybrid_mla_swa_moe_dit_layer"


def rmsnorm(x, g):
    xf = x.astype(jnp.float32)
    y = xf * lax.rsqrt(jnp.mean(xf * xf, axis=-1, keepdims=True) + NORM_EPS)
    return y.astype(x.dtype) * g


def ada_params(cond, w_ada, b_ada):
    m = jax.nn.silu(cond) @ w_ada + b_ada
    return jnp.split(m, N_MOD, axis=-1)


def modulate(x, g, shift, scale):
    return rmsnorm(x, g) * (1 + scale[..., None, :]) + shift[..., None, :]


def axial_rope_tables(row, col, dim, dtype):
    q = dim // 4
    freqs = ROPE_THETA ** (-jnp.arange(q, dtype=jnp.float32) / q)
    ang_r = row.astype(jnp.float32)[:, None] * freqs
    ang_c = col.astype(jnp.float32)[:, None] * freqs
    ang = jnp.concatenate([ang_r, ang_r, ang_c, ang_c], axis=-1)
    return jnp.cos(ang).astype(dtype), jnp.sin(ang).astype(dtype)


def rotate_axial(x):
    x1, x2, x3, x4 = jnp.split(x, 4, axis=-1)
    return jnp.concatenate([-x2, x1, -x4, x3], axis=-1)


def apply_rope(x, cos, sin):
    return x * cos[:, None, :] + rotate_axial(x) * sin[:, None, :]


def kv_side(h, p, rope):
    b, n = h.shape[:2]
    kv_lat, k_rope, k_swa, v_swa = jnp.split(h @ p['w_in'][:, :KV_COLS], KV_OFFSETS, axis=-1)
    kv = (rmsnorm(kv_lat, p['mla_kv_a_g']) @ p['w_kv_up']).reshape(b, n, MLA_HEADS, MLA_NOPE + MLA_V)
    k_nope, v_mla = kv[..., :MLA_NOPE], kv[..., MLA_NOPE:]
    k_r = jnp.broadcast_to(k_rope[:, :, None, :], (b, n, MLA_HEADS, MLA_ROPE))
    k_mla = rmsnorm(jnp.concatenate([k_nope, k_r], axis=-1), p['mla_k_g'])
    k_swa = rmsnorm(k_swa.reshape(b, n, SWA_KV_HEADS, SWA_HD), p['swa_k_g'])
    v_swa = v_swa.reshape(b, n, SWA_KV_HEADS, SWA_HD)
    if rope is not None:
        cos_m, sin_m, cos_s, sin_s = rope
        k_mla = jnp.concatenate([k_mla[..., :MLA_NOPE], apply_rope(k_mla[..., MLA_NOPE:], cos_m, sin_m)], axis=-1)
        k_swa = apply_rope(k_swa, cos_s, sin_s)
    return k_mla, v_mla, k_swa, v_swa


def q_side(h, p, rope):
    b, n = h.shape[:2]
    q_lat, q_swa, gate_a, gate_b = jnp.split(h @ p['w_in'][:, KV_COLS:], Q_OFFSETS, axis=-1)
    q_mla = (rmsnorm(q_lat, p['mla_q_a_g']) @ p['w_q_up']).reshape(b, n, MLA_HEADS, MLA_QK)
    q_mla = rmsnorm(q_mla, p['mla_q_g'])
    q_swa = rmsnorm(q_swa.reshape(b, n, SWA_HEADS, SWA_HD), p['swa_q_g'])
    if rope is not None:
        cos_m, sin_m, cos_s, sin_s = rope
        q_mla = jnp.concatenate([q_mla[..., :MLA_NOPE], apply_rope(q_mla[..., MLA_NOPE:], cos_m, sin_m)], axis=-1)
        q_swa = apply_rope(q_swa, cos_s, sin_s)
    return q_mla, q_swa, gate_a, gate_b


def dense_attend(q, k, v, scale):
    s = jnp.einsum('bqhd,bkhd->bhqk', q, k).astype(jnp.float32) * scale
    pr = jax.nn.softmax(s, axis=-1).astype(v.dtype)
    return jnp.einsum('bhqk,bkhd->bqhd', pr, v)


def mla_attend_latent(q, k_lat, v_lat, k_ctx, v_ctx):
    b, n = q.shape[:2]
    k = jnp.concatenate([k_lat, k_ctx], axis=1)
    v = jnp.concatenate([v_lat, v_ctx], axis=1)
    nb = n // Q_BLOCK
    qb = q.reshape(b, nb, Q_BLOCK, MLA_HEADS, MLA_QK).swapaxes(0, 1)
    o = lax.map(lambda qi: dense_attend(qi, k, v, MLA_QK ** -0.5), qb)
    return o.swapaxes(0, 1).reshape(b, n, MLA_HEADS * MLA_V)


def sink_logits(sink, lead_shape):
    s = sink.astype(jnp.float32).reshape(SWA_KV_HEADS, SWA_GROUP)[:, :, None, None]
    return jnp.broadcast_to(s, lead_shape + (1,))


def swa_attend_latent(q, k_lat, v_lat, k_ctx, v_ctx, sink):
    b, n = q.shape[:2]
    nb = n // BAND_BLOCK
    n_ctx = k_ctx.shape[1]
    nband = 3 * BAND_BLOCK
    qb = q.reshape(b, nb, BAND_BLOCK, SWA_KV_HEADS, SWA_GROUP, SWA_HD)

    def band(t):
        tp = jnp.pad(t.reshape(b, nb, BAND_BLOCK, SWA_KV_HEADS, SWA_HD), ((0, 0), (1, 1), (0, 0), (0, 0), (0, 0)))
        return jnp.concatenate([tp[:, :-2], tp[:, 1:-1], tp[:, 2:]], axis=2)

    kb, vb = band(k_lat), band(v_lat)
    scale = SWA_HD ** -0.5
    s_band = jnp.einsum('bnqkgd,bnjkd->bnkgqj', qb, kb).astype(jnp.float32) * scale
    qi = jnp.arange(BAND_BLOCK)[:, None]
    kj = jnp.arange(nband)[None, :]
    rel = qi + BAND_BLOCK - kj
    kpos = (jnp.arange(nb)[:, None, None] - 1) * BAND_BLOCK + kj[None]
    allowed = (jnp.abs(rel) <= WINDOW)[None] & (kpos >= 0) & (kpos < n)
    s_band = jnp.where(allowed[None, :, None, None], s_band, NEG_INF)
    s_ctx = jnp.einsum('bnqkgd,bckd->bnkgqc', qb, k_ctx).astype(jnp.float32) * scale
    s_all = jnp.concatenate([s_band, s_ctx, sink_logits(sink, s_band.shape[:-1])], axis=-1)
    pr = jax.nn.softmax(s_all, axis=-1).astype(v_lat.dtype)
    o = (jnp.einsum('bnkgqj,bnjkd->bnqkgd', pr[..., :nband], vb)
         + jnp.einsum('bnkgqc,bckd->bnqkgd', pr[..., nband:nband + n_ctx], v_ctx))
    return o.reshape(b, n, SWA_HEADS * SWA_HD)


def swa_attend_context(q, k_ctx, v_ctx, sink):
    b, n = q.shape[:2]
    qg = q.reshape(b, n, SWA_KV_HEADS, SWA_GROUP, SWA_HD)
    s = jnp.einsum('bqkgd,bckd->bkgqc', qg, k_ctx).astype(jnp.float32) * SWA_HD ** -0.5
    s_all = jnp.concatenate([s, sink_logits(sink, s.shape[:-1])], axis=-1)
    pr = jax.nn.softmax(s_all, axis=-1)[..., :n].astype(v_ctx.dtype)
    return jnp.einsum('bkgqc,bckd->bqkgd', pr, v_ctx).reshape(b, n, SWA_HEADS * SWA_HD)


def merge_branches(o_a, o_b, gate_a, gate_b, p):
    y = jax.nn.sigmoid(gate_a) * (o_a @ p['w_branch_a']) + jax.nn.sigmoid(gate_b) * (o_b @ p['w_branch_b'])
    return y @ p['w_out']


def clamped_swiglu(gu):
    x_glu, x_lin = gu[..., ::2], gu[..., 1::2]
    x_glu = jnp.minimum(x_glu, SWIGLU_LIMIT)
    x_lin = jnp.clip(x_lin, -SWIGLU_LIMIT, SWIGLU_LIMIT)
    return x_glu * jax.nn.sigmoid(SWIGLU_ALPHA * x_glu) * (x_lin + 1)


def moe(h, p):
    b, n, d = h.shape
    t = b * n
    a = t * TOP_K
    hf = h.reshape(t, d)
    logits = (hf @ p['w_router'] + p['b_router']).astype(jnp.float32)
    top_vals, top_idx = lax.top_k(logits, TOP_K)
    wts = jax.nn.softmax(top_vals, axis=-1).astype(h.dtype)
    expert_flat = top_idx.reshape(a).astype(jnp.int32)
    token_flat = jnp.arange(a, dtype=jnp.int32) // TOP_K
    w_flat = wts.reshape(a)
    order = jnp.argsort(expert_flat, stable=True)
    sorted_e = expert_flat[order]
    counts = jnp.bincount(expert_flat, length=N_EXPERTS)
    starts = jnp.cumsum(counts) - counts
    padded = (counts + MOE_BLOCK - 1) // MOE_BLOCK * MOE_BLOCK
    pends = jnp.cumsum(padded)
    pstarts = pends - padded
    dest = pstarts[sorted_e] + (jnp.arange(a, dtype=jnp.int32) - starts[sorted_e])
    n_slots = -(-(a + N_EXPERTS * (MOE_BLOCK - 1)) // MOE_BLOCK) * MOE_BLOCK
    nblk = n_slots // MOE_BLOCK
    slot_token = jnp.full((n_slots,), t, jnp.int32).at[dest].set(token_flat[order])
    slot_w = jnp.zeros((n_slots,), h.dtype).at[dest].set(w_flat[order])
    block_expert = jnp.clip(jnp.searchsorted(pends, jnp.arange(nblk) * MOE_BLOCK, side='right'), 0, N_EXPERTS - 1)
    x_pad = jnp.concatenate([hf, jnp.zeros((1, d), h.dtype)], axis=0)
    w_gu, b_gu, w_dn, b_dn = p['w_gate_up'], p['b_gate_up'], p['w_down'], p['b_down']

    def run_block(args):
        tok, e, w = args
        xb = x_pad[tok]
        act = clamped_swiglu(xb @ w_gu[e] + b_gu[e])
        return (act @ w_dn[e] + b_dn[e]) * w[:, None]

    out = lax.map(run_block, (slot_token.reshape(nblk, MOE_BLOCK), block_expert, slot_w.reshape(nblk, MOE_BLOCK)))
    y = jnp.zeros((t + 1, d), h.dtype).at[slot_token].add(out.reshape(n_slots, d))[:t]
    return y.reshape(b, n, d)


def hybrid_layer(x, ctx, mod, mod_ctx, p, rope, update_ctx):
    shift1, scale1, gate1, shift2, scale2, gate2 = mod
    cshift1, cscale1, cgate1, cshift2, cscale2, cgate2 = mod_ctx
    h = modulate(x, p['norm1_g'], shift1, scale1)
    hc = modulate(ctx, p['norm1_g'], cshift1, cscale1)
    k_mla_c, v_mla_c, k_swa_c, v_swa_c = kv_side(hc, p, None)
    k_mla, v_mla, k_swa, v_swa = kv_side(h, p, rope)
    q_mla, q_swa, gate_a, gate_b = q_side(h, p, rope)
    o_a = mla_attend_latent(q_mla, k_mla, v_mla, k_mla_c, v_mla_c)
    o_b = swa_attend_latent(q_swa, k_swa, v_swa, k_swa_c, v_swa_c, p['swa_sink'])
    x_new = x + gate1[..., None, :] * merge_branches(o_a, o_b, gate_a, gate_b, p)
    x_new = x_new + gate2[..., None, :] * moe(modulate(x_new, p['norm2_g'], shift2, scale2), p)
    if update_ctx:
        b, n_ctx = ctx.shape[:2]
        qc_mla, qc_swa, gca, gcb = q_side(hc, p, None)
        oc_a = dense_attend(qc_mla, k_mla_c, v_mla_c, MLA_QK ** -0.5).reshape(b, n_ctx, MLA_HEADS * MLA_V)
        oc_b = swa_attend_context(qc_swa, k_swa_c, v_swa_c, p['swa_sink'])
        ctx = ctx + cgate1[..., None, :] * merge_branches(oc_a, oc_b, gca, gcb, p)
        ctx = ctx + cgate2[..., None, :] * moe(modulate(ctx, p['norm2_g'], cshift2, cscale2), p)
    return x_new, ctx


def setup_inputs(seed: int = 0) -> dict:
    key = jax.random.key(seed)
    ks = jax.random.split(key, 32)
    f32 = jnp.float32

    def nrm(k, shape, scale):
        return jax.random.normal(k, shape, f32) * scale

    def gain(k, shape):
        return 1.0 + 0.02 * jax.random.normal(k, shape, f32)

    D, L, E, F = D_MODEL, DEPTH, N_EXPERTS, D_EXPERT
    return {
        "x": nrm(ks[0], (BATCH, SEQ, D), 1.0),
        "c": nrm(ks[1], (BATCH, D), 1.0),
        "ctx": nrm(ks[2], (BATCH, CTX_LEN, D), 1.0),
        "c_ctx": nrm(ks[3], (D,), 1.0),
        "w_ada": nrm(ks[4], (L, D, N_MOD * D), 0.5 * D ** -0.5),
        "b_ada": nrm(ks[5], (L, N_MOD * D), 0.02),
        "norm1_g": gain(ks[6], (L, D)),
        "norm2_g": gain(ks[7], (L, D)),
        "w_in": nrm(ks[8], (L, D, IN_COLS), D ** -0.5),
        "mla_q_a_g": gain(ks[9], (L, MLA_Q_RANK)),
        "mla_kv_a_g": gain(ks[10], (L, MLA_KV_RANK)),
        "w_q_up": nrm(ks[11], (L, MLA_Q_RANK, MLA_HEADS * MLA_QK), MLA_Q_RANK ** -0.5),
        "w_kv_up": nrm(ks[12], (L, MLA_KV_RANK, MLA_HEADS * (MLA_NOPE + MLA_V)), MLA_KV_RANK ** -0.5),
        "mla_q_g": gain(ks[13], (L, MLA_QK)),
        "mla_k_g": gain(ks[14], (L, MLA_QK)),
        "swa_q_g": gain(ks[15], (L, SWA_HD)),
        "swa_k_g": gain(ks[16], (L, SWA_HD)),
        "swa_sink": nrm(ks[17], (L, SWA_HEADS), 0.5),
        "w_branch_a": nrm(ks[18], (L, MLA_HEADS * MLA_V, D), (MLA_HEADS * MLA_V) ** -0.5),
        "w_branch_b": nrm(ks[19], (L, SWA_HEADS * SWA_HD, D), (SWA_HEADS * SWA_HD) ** -0.5),
        "w_out": nrm(ks[20], (L, D, D), D ** -0.5),
        "w_router": nrm(ks[21], (L, D, E), D ** -0.5),
        "b_router": nrm(ks[22], (L, E), 0.01),
        "w_gate_up": nrm(ks[23], (L, E, D, 2 * F), D ** -0.5),
        "b_gate_up": nrm(ks[24], (L, E, 2 * F), 0.02),
        "w_down": nrm(ks[25], (L, E, F, D), F ** -0.5),
        "b_down": nrm(ks[26], (L, E, D), 0.02),
    }


def reference(x, c, ctx, c_ctx, w_ada, b_ada, norm1_g, norm2_g, w_in, mla_q_a_g, mla_kv_a_g,
              w_q_up, w_kv_up, mla_q_g, mla_k_g, swa_q_g, swa_k_g, swa_sink, w_branch_a, w_branch_b,
              w_out, w_router, b_router, w_gate_up, b_gate_up, w_down, b_down):
    n = x.shape[1]
    ROWS = n // GRID_W
    row = jnp.repeat(jnp.arange(ROWS, dtype=jnp.int32), GRID_W)
    col = jnp.tile(jnp.arange(GRID_W, dtype=jnp.int32), ROWS)
    cos_m, sin_m = axial_rope_tables(row, col, MLA_ROPE, x.dtype)
    cos_s, sin_s = axial_rope_tables(row, col, SWA_HD, x.dtype)
    rope = (cos_m, sin_m, cos_s, sin_s)
    for l in range(DEPTH):
        p = dict(norm1_g=norm1_g[l], norm2_g=norm2_g[l], w_in=w_in[l], mla_q_a_g=mla_q_a_g[l],
                 mla_kv_a_g=mla_kv_a_g[l], w_q_up=w_q_up[l], w_kv_up=w_kv_up[l], mla_q_g=mla_q_g[l],
                 mla_k_g=mla_k_g[l], swa_q_g=swa_q_g[l], swa_k_g=swa_k_g[l], swa_sink=swa_sink[l],
                 w_branch_a=w_branch_a[l], w_branch_b=w_branch_b[l], w_out=w_out[l], w_router=w_router[l],
                 b_router=b_router[l], w_gate_up=w_gate_up[l], b_gate_up=b_gate_up[l], w_down=w_down[l],
                 b_down=b_down[l])
        mod = ada_params(c, w_ada[l], b_ada[l])
        mod_ctx = ada_params(c_ctx, w_ada[l], b_ada[l])
        x, ctx = hybrid_layer(x, ctx, mod, mod_ctx, p, rope, update_ctx=(l < DEPTH - 1))
    return x
```

```python
import contextlib
import numpy as np
import concourse.bass as bass
import concourse.mybir as mybir
from concourse.bass_utils import run_bass_kernel_spmd

F32 = mybir.dt.float32
BF16 = mybir.dt.bfloat16
AF = mybir.ActivationFunctionType
ALU = mybir.AluOpType
AX = mybir.AxisListType

ENGS = ("pe", "act", "dve", "pool", "sp")
D = 1024
NTOK = 4096
NCTX = 256
NKEY = NTOK + NCTX
NKT = NKEY // 128
NE = 32
EPS = 1e-6

C_BADA, C_G1, C_G2, C_C, C_QAG, C_KVAG, C_BGU = 0, 48, 56, 64, 80, 83, 85
NCOL = 85 + 512
R_QG, R_KG, R_SQG, R_SKG, R_BR, R_SINK, R_G1B, R_G2B = 0, 96, 192, 256, 320, 352, 1376, 2400
NROW = 3424


class Prog:
    def __init__(self, nc, same_engine_sync=True):
        self.nc = nc
        self.ops = []
        self.last_w = {}
        self.readers = {}
        self.same_engine_sync = same_engine_sync

    mute = False

    def op(self, eng, fn, reads=(), writes=(), stream=None):
        if self.mute:
            return None
        _ps = lambda r: r in ("pmod", "T0", "pm0", "pm1", "pkv", "T32") or (isinstance(r, tuple) and r[0] == "pb")
        writes = list(writes) + [r for r in reads if _ps(r)]
        reads = [r for r in reads if not _ps(r)]
        idx = len(self.ops)
        deps = set()
        for r in reads:
            if r in self.last_w:
                deps.add(self.last_w[r])
        for w in writes:
            if w in self.last_w:
                deps.add(self.last_w[w])
            for rd in self.readers.get(w, ()):
                deps.add(rd)
        for w in writes:
            self.last_w[w] = idx
            self.readers[w] = []
        for r in reads:
            self.readers.setdefault(r, []).append(idx)
        deps.discard(idx)
        self.ops.append(dict(eng=eng, fn=fn, deps=deps, stream=stream, inc=False))
        return idx

    def barrier(self):
        self.mute = False
        last = {}
        for i, o in enumerate(self.ops):
            if o["fn"] is None:
                continue
            key = ("s", o["stream"]) if o["stream"] is not None else ("e", o["eng"])
            last[key] = i
        deps = set(last.values())
        for e in ENGS:
            self.ops.append(dict(eng=e, fn=None, deps=set(deps), stream=None, inc=False))
        self.last_w = {}
        self.readers = {}

    def _skip(self, od, o):
        return (od["stream"] is None and o["stream"] is None and od["eng"] == o["eng"]
                and (od["eng"] == "pe" or not self.same_engine_sync))

    def emit(self):
        nc = self.nc
        ops = self.ops
        for o in ops:
            for d in o["deps"]:
                od = ops[d]
                if od["stream"] is None and not self._skip(od, o):
                    od["inc"] = True
        cnt = {}
        for o in ops:
            if o["stream"] is not None:
                k = ("s", o["stream"])
                cnt[k] = cnt.get(k, 0) + 16
                o["done"] = (k, cnt[k])
            elif o["inc"]:
                k = ("e", o["eng"])
                cnt[k] = cnt.get(k, 0) + 1
                o["done"] = (k, cnt[k])
            else:
                o["done"] = None
        self.final_counts = dict(cnt)
        with contextlib.ExitStack() as st:
            sems = {}
            for k in cnt:
                sems[k] = st.enter_context(nc.semaphore(f"sem_{k[0]}_{k[1]}"))
            block = st.enter_context(nc.Block())

            def run_engine(ename, handle):
                known = {}
                for o in ops:
                    if o["eng"] != ename:
                        continue
                    need = {}
                    for d in o["deps"]:
                        od = ops[d]
                        if od["done"] is None or self._skip(od, o):
                            continue
                        k, v = od["done"]
                        if need.get(k, 0) < v:
                            need[k] = v
                    for k, v in need.items():
                        if known.get(k, 0) < v:
                            handle.wait_ge(sems[k], v)
                            known[k] = v
                    if o["fn"] is None:
                        continue
                    ins = o["fn"](handle)
                    if o["done"] is not None:
                        k, v = o["done"]
                        ins.then_inc(sems[k], 16 if k[0] == "s" else 1)
                if ename == "sp":
                    for k, v in cnt.items():
                        if k[0] == "s" and known.get(k, 0) < v:
                            handle.wait_ge(sems[k], v)

            @block.tensor
            def _(e):
                run_engine("pe", e)

            @block.scalar
            def _(e):
                run_engine("act", e)

            @block.vector
            def _(e):
                run_engine("dve", e)

            @block.gpsimd
            def _(e):
                run_engine("pool", e)

            @block.sync
            def _(e):
                run_engine("sp", e)


class Alloc:
    def __init__(self, nc):
        self.nc = nc
        self.top = (int(nc._sbuf_addr_for_side("left")) + 63) // 64 * 64
        self.limit = int(nc._sbuf_addr_for_side("right")) // 64 * 64
        self.n = 0

    def __call__(self, shape, dt, name=None):
        per = int(np.prod(shape[1:])) * (2 if dt == BF16 else 4)
        per = (per + 63) // 64 * 64
        off = self.top
        self.top += per
        assert self.top <= self.limit, f"SBUF overflow {self.top}"
        self.n += 1
        return self.nc.alloc_sbuf_tensor_at(name or f"t{self.n}_{off}", list(shape), dt, offset=off)

    def mark(self):
        return self.top

    def reset(self, m):
        self.top = m


def bcast(ap, axis, shape):
    return ap.unsqueeze(axis).to_broadcast(list(shape))


def build(stage=99, dbg=False):
    nc = bass.Bass("TRN2", target_bir_lowering=False)
    P = Prog(nc)

    def din(name, shape, dt=F32):
        return nc.dram_tensor(name, list(shape), dt, kind="ExternalInput").ap()

    def dscr(name, shape, dt):
        return nc.dram_tensor(name, list(shape), dt, kind=("ExternalOutput" if dbg else "Internal")).ap()

    x_d = din("x", [NTOK, D])
    ctx_d = din("ctx", [NCTX, D])
    cols_d = din("cols", [128, NCOL])
    rows_d = din("rows", [128, NROW])
    rope_d = din("rope", [32, 128, 192])
    mask_d = din("masks", [128, 256])
    wada_d = din("w_ada", [D, 6 * D])
    win_d = din("w_in", [D, 3488])
    wqup_d = din("w_q_up", [384, 768])
    wkvup_d = din("w_kv_up", [256, 1024])
    wba_d = din("w_branch_a", [512, D])
    wbb_d = din("w_branch_b", [512, D])
    wout_d = din("w_out", [D, D])
    wr_d = din("w_router", [D, NE])
    wgu_d = din("w_gu", [NE, D, 2 * D])
    wdn_d = din("w_dn", [NE, D, D])
    bdn_d = din("b_dn", [NE, D])
    out_d = nc.dram_tensor("out", [NTOK, D], F32, kind="ExternalOutput").ap()

    s_kt = dscr("s_kt", [96, 8, NKEY], BF16)
    s_va = dscr("s_va", [8, 128, NKT, 65], BF16)
    s_kst = dscr("s_kst", [128, NKEY], BF16)
    s_vsa = dscr("s_vsa", [128, NKT, 130], BF16)
    s_qm = dscr("s_qm", [96, 8, NTOK], BF16)
    s_qs = dscr("s_qs", [128, 32, 512], BF16)
    s_gate = dscr("s_gate", [128, 16, NTOK], BF16)
    s_o = dscr("s_o", [64, 16, NTOK], BF16)
    s_xnew = dscr("s_xnew", [NTOK, D], F32)
    s_h2 = dscr("s_h2", [128, 8, NTOK], BF16)
    if dbg:
        s_wt = dscr("s_wt", [32, NTOK], F32)

    A = Alloc(nc)
    T0t = nc.alloc_psum_tensor("t0", [128, 8, 128], BF16)
    T0b = T0t[:, :, :]
    PB = nc.alloc_psum_tensor("pb", [128, 7, 512], F32)

    def bank2(i):
        return PB[:, i:i + 2, :].rearrange("p a b -> p (a b)")

    def dma(eng, out, in_, reads, writes, stream):
        P.op(eng, lambda e: e.dma_start(out=out, in_=in_), reads=reads, writes=writes, stream=stream)

    def mm(out, lhsT, rhs, start, stop, reads, writes):
        P.op("pe", lambda e: e.matmul(out, lhsT=lhsT, rhs=rhs, start=start, stop=stop), reads=reads, writes=writes)

    def tr(out, in_, ident, reads, writes):
        P.op("pe", lambda e: e.transpose(out=out, in_=in_, identity=ident), reads=reads, writes=writes)

    def act(out, in_, func, reads, writes, **kw):
        P.op("act", lambda e: e.activation(out=out, in_=in_, func=func, **kw), reads=reads, writes=writes)

    def tt(eng, out, in0, in1, op, reads, writes):
        P.op(eng, lambda e: e.tensor_tensor(out=out, in0=in0, in1=in1, op=op), reads=reads, writes=writes)

    def ts(eng, out, in0, s1, s2, op0, op1, reads, writes):
        if s2 is None:
            P.op(eng, lambda e: e.tensor_scalar(out=out, in0=in0, scalar1=s1, scalar2=None, op0=op0), reads=reads, writes=writes)
        else:
            P.op(eng, lambda e: e.tensor_scalar(out=out, in0=in0, scalar1=s1, scalar2=s2, op0=op0, op1=op1), reads=reads, writes=writes)

    def stt(eng, out, in0, scalar, in1, op0, op1, reads, writes):
        P.op(eng, lambda e: e.scalar_tensor_tensor(out=out, in0=in0, scalar=scalar, in1=in1, op0=op0, op1=op1),
             reads=reads, writes=writes)

    def cp(eng, out, in_, reads, writes):
        if eng == "act":
            P.op("act", lambda e: e.copy(out=out, in_=in_), reads=reads, writes=writes)
        else:
            P.op(eng, lambda e: e.tensor_copy(out=out, in_=in_), reads=reads, writes=writes)

    def rsqrt(src, dst, n, reads, writes, tag):
        act(dst, src, AF.Sqrt, reads, writes, scale=1.0 / n, bias=EPS)
        P.op("dve", lambda e: e.reciprocal(out=dst, in_=dst), reads=writes, writes=writes)

    cols_t = A([128, NCOL], F32, "cols_t")
    g2bc = A([128, D], F32, "g2bc")
    WT = A([32, NTOK], F32, "WT")
    WTb = A([32, NTOK], BF16, "WTb")
    identf = A([128, 128], F32, "identf")
    ident = A([128, 128], BF16, "ident")
    eps_t = A([128, 1], F32, "eps_t")
    gs2 = A([128, 8], F32, "gs2")
    sh2 = A([128, 8], F32, "sh2")
    markA = A.mark()
    rows_t = A([128, NROW], F32, "rows_t")
    g1bc = A([128, D], F32, "g1bc")
    modT = A([128, 48, 2], F32, "modT")
    gs1 = A([128, 8, 2], F32, "gs1")
    onesb = A([128, 128], BF16, "onesb")
    onesf = A([128, 64], F32, "onesf")
    maskb = A([128, 256], BF16, "maskb")
    markB = A.mark()

    dma("sp", cols_t[:], cols_d, [], ["cols"], 0)
    dma("sp", rows_t[:], rows_d, [], ["rows"], 0)
    maskf = A([128, 256], F32)
    dma("sp", maskf[:], mask_d, [], ["maskf"], 0)
    P.op("pool", lambda e: e.memset(identf[:], 0.0), writes=["identf"])
    P.op("pool", lambda e: e.affine_select(out=identf[:], in_=identf[:], pattern=[[-1, 128]], compare_op=ALU.not_equal,
                                            fill=1.0, base=0, channel_multiplier=1), reads=["identf"], writes=["identf"])
    cp("dve", ident[:], identf[:], ["identf"], ["ident"])
    cp("dve", maskb[:], maskf[:], ["maskf"], ["maskb"])
    P.op("pool", lambda e: e.memset(onesb[:], 1.0), writes=["onesb"])
    P.op("pool", lambda e: e.memset(onesf[:], 1.0), writes=["onesf"])
    P.op("pool", lambda e: e.memset(eps_t[:], EPS), writes=["eps"])
    sc = A([128, 8, 2], F32)
    scB = A([128, 8, 128], F32)
    act(sc[:, :, 0], cols_t[:, C_C:C_C + 8], AF.Silu, ["cols"], ["sc0"])
    act(sc[:, :, 1], cols_t[:, C_C + 8:C_C + 16], AF.Silu, ["cols"], ["sc1"])
    cp("dve", scB[:], sc[:, :, 0:1].to_broadcast([128, 8, 128]), ["sc0"], ["scB"])
    wa = [A([128, 8, 512], F32) for _ in range(2)]
    wada_v = wada_d.rearrange("(k p) n -> p k n", p=128)
    pmod = PB[:, 0, 0:96].rearrange("p (j s) -> p j s", s=2)
    bcbank = {4: 1, 5: 2, 10: 3, 11: 4}
    for cc in range(12):
        b = cc % 2
        dma("sp", wa[b][:], wada_v[:, :, cc * 512:(cc + 1) * 512], [], [("wa", b)], 0)
        for jj in range(4):
            j = cc * 4 + jj
            for k in range(8):
                mm(pmod[:, j, :], wa[b][:, k, jj * 128:(jj + 1) * 128], sc[:, k, :], k == 0, k == 7,
                   [("wa", b), "sc0", "sc1"], ["pmod"])
        if cc in bcbank:
            bk = bcbank[cc]
            for k in range(8):
                mm(PB[:, bk, :], scB[:, k, :], wa[b][:, k, :], k == 0, k == 7, [("wa", b), "scB"], [("pb", bk)])
            half = cc % 2
            if cc < 6:
                tt("dve", g1bc[:, half * 512:(half + 1) * 512], PB[:, bk, :], rows_t[:, R_G1B + half * 512:R_G1B + (half + 1) * 512],
                   ALU.add, [("pb", bk), "rows"], [("g1bc", half)])
            else:
                tt("dve", g2bc[:, half * 512:(half + 1) * 512], PB[:, bk, :], rows_t[:, R_G2B + half * 512:R_G2B + (half + 1) * 512],
                   ALU.add, [("pb", bk), "rows"], [("g2bc", half)])
    tt("dve", modT[:], pmod, bcast(cols_t[:, C_BADA:C_BADA + 48], 2, [128, 48, 2]), ALU.add, ["pmod", "cols"], ["modT"])
    stt("dve", gs1[:], modT[:, 8:16, :], 1.0, bcast(cols_t[:, C_G1:C_G1 + 8], 2, [128, 8, 2]), ALU.add, ALU.mult,
        ["modT", "cols"], ["gs1"])
    stt("dve", gs2[:], modT[:, 32:40, 0], 1.0, cols_t[:, C_G2:C_G2 + 8], ALU.add, ALU.mult, ["modT", "cols"], ["gs2"])
    cp("dve", sh2[:], modT[:, 24:32, 0], ["modT"], ["sh2"])
    P.barrier()
    A.reset(markB)

    if stage >= 1:
        win = A([128, 8, 3488], BF16, "win")
        wkv = A([128, 2, 1024], BF16, "wkv")
        wq = A([128, 3, 768], BF16, "wq")
        win_v = win_d.rearrange("(k p) n -> p k n", p=128)
        import os as _os
        SKIP = _os.environ.get('K_SKIP', '')
        if 'w' in SKIP:
            P.mute = True
        for k in range(8):
            for c4 in range(4):
                dma("pool", win[:, k, c4 * 872:(c4 + 1) * 872], win_v[:, k, c4 * 872:(c4 + 1) * 872], [], [("win", k)], 1)
        dma("pool", wkv[:], wkvup_d.rearrange("(k p) n -> p k n", p=128), [], ["wkv"], 1)
        dma("pool", wq[:], wqup_d.rearrange("(k p) n -> p k n", p=128), [], ["wq"], 1)
        WIN = [("win", k) for k in range(8)]
        P.mute = False
        xt = [A([128, D], F32) for _ in range(2)]
        junk = A([128, D], F32)
        ssq = A([128, 1], F32)
        rstd = A([128, 1], F32)
        xn = A([128, D], BF16)
        tmpf = A([128, 8, 128], F32)
        hT = [A([128, 8, 512], BF16) for _ in range(1)]
        latf = A([128, 5, 512], F32)
        sq = A([128, 5, 512], BF16)
        rbc = A([128, 512], F32)
        latn = A([128, 5, 512], BF16)
        sg = A([128, 8, 512], BF16)
        ropet = [A([128, 192], F32) for _ in range(2)]
        scr = A([128, 768], F32)
        scr2 = A([128, 768], F32)
        rtA = A([128, 512], F32)
        rtB = A([128, 512], F32)
        st8 = A([128, 8], F32)
        ssqr = A([128, 1], F32)
        kn = A([128, 8, 96], BF16)
        krg = A([128, 32], F32)
        krr = A([128, 32], F32)
        ktile = A([128, 8, 128], BF16)
        vtile = [A([128, 8, 65], BF16) for _ in range(2)]
        qb = A([128, 8, 96], BF16)
        qtile = A([128, 8, 128], BF16)
        ksb = A([128, 128], BF16)
        kstile = A([128, 128], BF16)
        vstile = [A([128, 2, 65], BF16) for _ in range(2)]
        qsb = A([128, 8, 64], BF16)
        qsbP = A([128, 4, 2, 64], BF16)
        qstile = A([128, 4, 128], BF16)
        for b in range(0 if 'm' not in SKIP else 2, 2):
            P.op("pool", lambda e, b=b: e.memset(vtile[b][:], 1.0), writes=[("vtile", b)])
            P.op("pool", lambda e, b=b: e.memset(vstile[b][:], 1.0), writes=[("vstile", b)])

        def rope(src3, H, R, cos, ss, dst3, rd, wr):
            q = R // 4
            a3 = rtA[:, 0:H * R].rearrange("p (h r) -> p h r", h=H)
            b3 = rtB[:, 0:H * R].rearrange("p (h r) -> p h r", h=H)
            tt("pool", a3, src3, bcast(cos, 1, [128, H, R]), ALU.mult, rd, ["rtA"])
            s5 = src3.rearrange("p h (a b c) -> p h a b c", a=2, b=2)
            b5 = b3.rearrange("p h (a b c) -> p h a b c", a=2, b=2)
            ss4 = ss.rearrange("p (a b c) -> p a b c", a=2, b=2)
            tt("pool", b5[:, :, :, 0, :], s5[:, :, :, 1, :], bcast(ss4[:, :, 0, :], 1, [128, H, 2, q]), ALU.mult, rd, ["rtB0"])
            tt("pool", b5[:, :, :, 1, :], s5[:, :, :, 0, :], bcast(ss4[:, :, 1, :], 1, [128, H, 2, q]), ALU.mult, rd, ["rtB1"])
            tt("pool", dst3, a3, b3, ALU.add, ["rtA", "rtB0", "rtB1"], wr)

        def headnorm(src3, H, Dh, gain, dstf3, rd, tag, extra=None):
            s3 = scr[:, 0:H * Dh].rearrange("p (h d) -> p h d", h=H)
            act(s3, src3, AF.Square, rd, ["scr"])
            P.op("dve", lambda e: e.reduce_sum(out=st8[:, 0:H], in_=s3, axis=AX.X), reads=["scr"], writes=["st8"])
            if extra is not None:
                ts("dve", st8[:, 0:H], st8[:, 0:H], extra[0], None, ALU.add, None, ["st8", extra[1]], ["st8"])
            rsqrt(st8[:, 0:H], st8[:, 0:H], float(Dh if extra is None else 96), ["st8"], ["st8"], tag)
            tt("dve", s3, src3, bcast(st8[:, 0:H], 2, [128, H, Dh]), ALU.mult, rd + ["st8", "scr"], ["scr"])
            tt("pool", dstf3, s3, bcast(gain, 1, [128, H, Dh]), ALU.mult, ["scr", "rows"], [tag])

        supers = [(ctx_d, 0, 2, 1, False, 0)] + [(x_d, i * 512, 4, 0, True, 2 + 4 * i) for i in range(8)]
        import os as _os
        supers = supers[:int(_os.environ.get('K_NSUP', '9'))]
        CUT = int(_os.environ.get('K_CUT', '99'))
        tile_ctr = 0
        for si, (src, row0, NT, sidx, latent, kt0) in enumerate(supers):
            NTK = NT * 128
            hb = 0
            for t in range(NT):
                b = tile_ctr % 2
                tile_ctr += 1
                dma("sp", xt[b][:], src[row0 + t * 128:row0 + (t + 1) * 128, :], [], [("xt", b)], 2)
                act(junk[:], xt[b][:], AF.Square, [("xt", b)], ["junk", "ssq"], accum_out=ssq[:])
                rsqrt(ssq[:], rstd[:], 1024.0, ["ssq"], ["rstd"], "x")
                act(xn[:], xt[b][:], AF.Copy, [("xt", b), "rstd"], ["xn"], scale=rstd[:, 0:1])
                for k in range(8):
                    tr(T0b[:, k, :], xn[:, k * 128:(k + 1) * 128], ident[:], ["xn", "ident"], ["T0"])
                tt("dve", tmpf[:], T0b, bcast(gs1[:, :, sidx], 2, [128, 8, 128]), ALU.mult, ["T0", "gs1"], ["tmpf"])
                tt("dve", hT[hb][:, :, t * 128:(t + 1) * 128], tmpf[:], bcast(modT[:, 0:8, sidx], 2, [128, 8, 128]), ALU.add,
                   ["tmpf", "modT"], [("hT", hb, t)])
            HT = [("hT", hb, t) for t in range(NT)]
            if CUT == 1:
                P.mute = True
            chunks = [(0, 0), (1, 128)] + ([(2, 544), (3, 672), (4, 800)] if latent else [])
            fb = 0
            for ci, c0 in chunks:
                bk = 1 + (fb % 2)
                fb += 1
                for k in range(8):
                    mm(PB[:, bk, 0:NTK], win[:, k, c0:c0 + 128], hT[hb][:, k, 0:NTK], k == 0, k == 7, HT + [("win", k)], [("pb", bk)])
                cp("dve", latf[:, ci, 0:NTK], PB[:, bk, 0:NTK], [("pb", bk)], [("latf", ci)])
                act(sq[:, ci, 0:NTK], PB[:, bk, 0:NTK], AF.Square, [("pb", bk)], [("sq", ci)])
            for (cis, n, goff) in ([([0, 1], 256.0, C_KVAG)] + ([([2, 3, 4], 384.0, C_QAG - 2)] if latent else [])):
                bk = 1 + (fb % 2)
                fb += 1
                for i, ci in enumerate(cis):
                    mm(PB[:, bk, 0:NTK], onesb[:], sq[:, ci, 0:NTK], i == 0, i == len(cis) - 1, [("sq", ci), "onesb"], [("pb", bk)])
                rsqrt(PB[:, bk, 0:NTK], rbc[:, 0:NTK], n, [("pb", bk)], ["rbc"], "lat")
                for ci in cis:
                    stt("dve", latn[:, ci, 0:NTK], latf[:, ci, 0:NTK], cols_t[:, goff + ci:goff + ci + 1], rbc[:, 0:NTK],
                        ALU.mult, ALU.mult, [("latf", ci), "rbc", "cols"], [("latn", ci)])
            if latent:
                for gc in range(16):
                    bk = 1 + (fb % 2)
                    fb += 1
                    c0 = 1440 + gc * 128
                    for k in range(8):
                        mm(PB[:, bk, :], win[:, k, c0:c0 + 128], hT[hb][:, k, :], k == 0, k == 7, HT + [("win", k)], [("pb", bk)])
                    act(sg[:, gc % 8, :], PB[:, bk, :], AF.Sigmoid, [("pb", bk)], [("sg", gc % 8)])
                    if gc % 8 == 7:
                        g0 = gc - 7
                        dma("sp", s_gate[:, g0:g0 + 8, row0:row0 + 512], sg[:], [("sg", i) for i in range(8)], [("sgall")], 3)
            if CUT == 2:
                P.mute = True
            pm = bank2(3)
            pkv = bank2(5)
            for t in range(NT):
                tsl = slice(t * 128, (t + 1) * 128)
                kt = kt0 + t
                key0 = kt * 128
                tok0 = row0 + t * 128
                vb = kt % 2
                if latent:
                    rb = kt % 2
                    dma("sp", ropet[rb][:], rope_d[kt - 2], [], [("rope", rb)], 2)
                    cos_m, ss_m = ropet[rb][:, 0:32], ropet[rb][:, 32:64]
                    cos_s, ss_s = ropet[rb][:, 64:128], ropet[rb][:, 128:192]
                    RP = [("rope", rb)]
                for k in range(8):
                    mm(pm[:, 0:288], hT[hb][:, k, tsl], win[:, k, 256:544], k == 0, k == 7, [("hT", hb, t), ("win", k)], ["pm0"])
                if latent:
                    for k in range(8):
                        mm(pm[:, 512:1024], hT[hb][:, k, tsl], win[:, k, 928:1440], k == 0, k == 7, [("hT", hb, t), ("win", k)], ["pm1"])
                for half in range(2):
                    for c in range(2):
                        mm(pkv[:, half * 512:(half + 1) * 512], latn[:, c, tsl], wkv[:, c, half * 512:(half + 1) * 512], c == 0, c == 1,
                           [("latn", c), "wkv"], ["pkv"])
                pkv3 = pkv.rearrange("p (h d) -> p h d", h=8)
                if CUT == 3:
                    P.mute = True
                act(junk[:, 0:32], pm[:, 0:32], AF.Square, ["pm0"], ["junk", "ssqr"], accum_out=ssqr[:])
                s3 = scr[:, 0:512].rearrange("p (h d) -> p h d", h=8)
                act(s3, pkv3[:, :, 0:64], AF.Square, ["pkv"], ["scr"])
                P.op("dve", lambda e, s3=s3: e.reduce_sum(out=st8[:], in_=s3, axis=AX.X), reads=["scr"], writes=["st8"])
                ts("dve", st8[:], st8[:], ssqr[:, 0:1], None, ALU.add, None, ["st8", "ssqr"], ["st8"])
                rsqrt(st8[:], st8[:], 96.0, ["st8"], ["st8"], "k")
                tt("dve", s3, pkv3[:, :, 0:64], bcast(st8[:], 2, [128, 8, 64]), ALU.mult, ["pkv", "st8", "scr"], ["scr"])
                tt("pool", kn[:, :, 0:64], s3, bcast(rows_t[:, R_KG:R_KG + 64], 1, [128, 8, 64]), ALU.mult, ["scr", "rows"], ["kn0"])
                tt("dve", krg[:], pm[:, 0:32], rows_t[:, R_KG + 64:R_KG + 96], ALU.mult, ["pm0", "rows"], ["krg"])
                if latent:
                    rope(krg[:].unsqueeze(1), 1, 32, cos_m, ss_m, krr[:].unsqueeze(1), ["krg"] + RP, ["krr"])
                    krsrc, KR = krr, "krr"
                else:
                    krsrc, KR = krg, "krg"
                tt("dve", kn[:, :, 64:96], bcast(krsrc[:], 1, [128, 8, 32]), bcast(st8[:], 2, [128, 8, 32]), ALU.mult, [KR, "st8"], ["kn1"])
                for h in range(8):
                    tr(T0b[0:96, h, :], kn[:, h, :], ident[:], ["kn0", "kn1", "ident"], ["T0"])
                cp("act", ktile[0:96, :, :], T0b[0:96, :, :], ["T0"], ["ktile"])
                dma("sp", s_kt[:, :, key0:key0 + 128], ktile[0:96, :, :], ["ktile"], [], 3)
                cp("act", vtile[vb][:, :, 0:64], pkv3[:, :, 64:128], ["pkv"], [("vtile", vb)])
                dma("sp", s_va.rearrange("h p t e -> p h t e")[:, :, kt, :], vtile[vb][:], [("vtile", vb)], [], 3)
                if CUT == 4:
                    P.mute = True
                pks = pm[:, 32:160].rearrange("p (h d) -> p h d", h=2)
                ksn = scr2[:, 0:128].rearrange("p (h d) -> p h d", h=2)
                headnorm(pks, 2, 64, rows_t[:, R_SKG:R_SKG + 64], ksn, ["pm0"], "scr2")
                ksb3 = ksb[:].rearrange("p (h d) -> p h d", h=2)
                if latent:
                    rope(ksn, 2, 64, cos_s, ss_s, ksb3, ["scr2"] + RP, ["ksb"])
                else:
                    cp("pool", ksb3, ksn, ["scr2"], ["ksb"])
                tr(T0b[:, 0, :], ksb[:], ident[:], ["ksb", "ident"], ["T0"])
                cp("act", kstile[:], T0b[:, 0, :], ["T0"], ["kstile"])
                dma("sp", s_kst[:, key0:key0 + 128], kstile[:], ["kstile"], [], 3)
                cp("act", vstile[vb][:, :, 0:64], pm[:, 160:288].rearrange("p (h d) -> p h d", h=2), ["pm0"], [("vstile", vb)])
                dma("sp", s_vsa[:, kt, :], vstile[vb][:].rearrange("p h e -> p (h e)"), [("vstile", vb)], [], 3)
                if not latent:
                    continue
                pq = pkv
                for (c0, c1) in ((0, 512), (512, 768)):
                    for c in range(3):
                        mm(pq[:, c0:c1], latn[:, 2 + c, tsl], wq[:, c, c0:c1], c == 0, c == 2, [("latn", 2 + c), "wq"], ["pkv"])
                pq3 = pq[:, 0:768].rearrange("p (h d) -> p h d", h=8)
                qn3 = scr2[:, 0:768].rearrange("p (h d) -> p h d", h=8)
                headnorm(pq3, 8, 96, rows_t[:, R_QG:R_QG + 96], qn3, ["pkv"], "scr2")
                cp("pool", qb[:, :, 0:64], qn3[:, :, 0:64], ["scr2"], ["qb0"])
                rope(qn3[:, :, 64:96], 8, 32, cos_m, ss_m, qb[:, :, 64:96], ["scr2"] + RP, ["qb1"])
                for h in range(8):
                    tr(T0b[0:96, h, :], qb[:, h, :], ident[:], ["qb0", "qb1", "ident"], ["T0"])
                cp("act", qtile[0:96, :, :], T0b[0:96, :, :], ["T0"], ["qtile"])
                dma("sp", s_qm[:, :, tok0:tok0 + 128], qtile[0:96, :, :], ["qtile"], [], 3)
                pqs = pm[:, 512:1024].rearrange("p (h d) -> p h d", h=8)
                qsn = scr2[:, 0:512].rearrange("p (h d) -> p h d", h=8)
                headnorm(pqs, 8, 64, rows_t[:, R_SQG:R_SQG + 64], qsn, ["pm1"], "scr2")
                rope(qsn, 8, 64, cos_s, ss_s, qsb[:], ["scr2"] + RP, ["qsb"])
                cp("pool", qsbP[:].rearrange("p hg g d -> p g hg d"), qsb[:].rearrange("p (g hg) d -> p g hg d", g=2), ["qsb"], ["qsbP"])
                for hg in range(4):
                    tr(T0b[:, hg, :], qsbP[:, hg, :, :].rearrange("p g d -> p (g d)"), ident[:], ["qsbP", "ident"], ["T0"])
                cp("act", qstile[:], T0b[:, 0:4, :], ["T0"], ["qstile"])
                dma("sp", s_qs[:, kt - 2, :], qstile[:].rearrange("p h q -> p (h q)"), ["qstile"], [], 3)
        P.barrier()
        A.reset(markB)

    if stage >= 2:
        kst = A([128, NKEY], BF16, "kst")
        vsa = A([128, NKT, 130], BF16, "vsa")
        esk = A([128, 1024], F32, "esk")
        dma("sp", kst[:], s_kst, [], ["kst"], 2)
        dma("sp", vsa[:], s_vsa, [], ["vsa"], 2)
        act(esk[:], rows_t[:, R_SINK:R_SINK + 1024], AF.Exp, ["rows"], ["esk"])
        kth = [A([128, NKEY], BF16) for _ in range(2)]
        vah = [A([128, NKT, 65], BF16) for _ in range(2)]
        qm = A([128, 8, 512], BF16)
        qs = A([128, 4, 512], BF16)
        PT = [A([128, 512], BF16) for _ in range(3)]
        oT = A([64, 16, 512], BF16)
        osb = A([64, 512], F32)
        rden = A([128, 512], F32)
        sctr = [0]
        octr = [0]
        hctr = 0

        def attend(tiles, qk_fn, pv_fn, masks, tag):
            n = len(tiles)
            ob = 3 + (octr[0] % 2)
            octr[0] += 1
            slots = []

            def issue_qk(i):
                sbk = sctr[0] % 3
                sctr[0] += 1
                qk_fn(tiles[i], PB[:, sbk, :], ("pb", sbk))
                slots.append(sbk)

            issue_qk(0)
            if n > 1:
                issue_qk(1)
            for i in range(n):
                sbk = slots[i]
                act(PT[sbk][:], PB[:, sbk, :], AF.Exp, [("pb", sbk)], [("PT", sbk)], scale=tag)
                if masks[i] is not None:
                    m0 = masks[i]
                    p3 = PT[sbk][:].rearrange("p (h q) -> p h q", h=4)
                    tt("pool", p3, p3, bcast(maskb[:, m0:m0 + 128], 1, [128, 4, 128]), ALU.mult, [("PT", sbk), "maskb"], [("PT", sbk)])
                pv_fn(tiles[i], PB[0:65, ob, :], PT[sbk][:], i == 0, i == n - 1, ("PT", sbk), ("pb", ob))
                if i + 2 < n:
                    issue_qk(i + 2)
            return ob

        def finish(ob, dst, add_sink=None):
            if add_sink is not None:
                tt("dve", rden[64:65, :], PB[64:65, ob, :], add_sink, ALU.add, [("pb", ob), "esk"], ["rden"])
                P.op("dve", lambda e: e.reciprocal(out=rden[64:65, :], in_=rden[64:65, :]), reads=["rden"], writes=["rden"])
            else:
                P.op("dve", lambda e: e.reciprocal(out=rden[64:65, :], in_=PB[64:65, ob, :]), reads=[("pb", ob)], writes=["rden"])
            mm(PB[0:64, 5, :], onesf[64:65, 0:64], rden[64:65, :], True, True, ["rden", "onesf"], [("pb", 5)])
            cp("act", osb[:], PB[0:64, ob, :], [("pb", ob)], ["osb"])
            tt("dve", dst, osb[:] if len(dst.shape) == 2 else osb[:].rearrange("p (h q) -> p h q", h=4),
               PB[0:64, 5, :] if len(dst.shape) == 2 else PB[0:64, 5, :].rearrange("p (h q) -> p h q", h=4),
               ALU.mult, ["osb", ("pb", 5)], ["oT"])

        for ch in range(8):
            tok0 = ch * 512
            dma("sp", qm[0:96, :, :], s_qm[:, :, tok0:tok0 + 512], [], ["qm"], 2)
            dma("sp", qs[:], s_qs[:, ch * 4:(ch + 1) * 4, :], [], ["qs"], 2)
            for h in range(8):
                kb = hctr % 2
                hctr += 1
                dma("sp", kth[kb][0:96, :], s_kt[:, h, :], [], [("kth", kb)], 2)
                dma("sp", vah[kb][:], s_va[h], [], [("vah", kb)], 2)

                def qk(kt, S, sres, h=h, kb=kb):
                    mm(S, kth[kb][0:96, kt * 128:(kt + 1) * 128], qm[0:96, h, :], True, True, [("kth", kb), "qm"], [sres])

                def pv(kt, O, Pt, st, sp_, pres, ores, kb=kb):
                    mm(O, vah[kb][:, kt, :], Pt, st, sp_, [("vah", kb), pres], [ores])

                ob = attend(list(range(NKT)), qk, pv, [None] * NKT, 96 ** -0.5)
                finish(ob, oT[:, h, :])
            for g in range(2):
                for qbk in range(4):
                    blk = ch * 4 + qbk
                    tiles, masks = [0, 1], [None, None]
                    for kbk, mk in ((blk - 1, 0), (blk, None), (blk + 1, 128)):
                        if 0 <= kbk < 32:
                            tiles.append(2 + kbk)
                            masks.append(mk)
                    gs_ = slice(g * 64, (g + 1) * 64)

                    def qk(kt, S, sres, gs_=gs_, qbk=qbk):
                        mm(S, kst[gs_, kt * 128:(kt + 1) * 128], qs[gs_, qbk, :], True, True, ["kst", "qs"], [sres])

                    def pv(kt, O, Pt, st, sp_, pres, ores, g=g):
                        mm(O, vsa[:, kt, g * 65:(g + 1) * 65], Pt, st, sp_, ["vsa", pres], [ores])

                    ob = attend(tiles, qk, pv, masks, 0.125)
                    finish(ob, oT[:, 8 + g * 4:8 + g * 4 + 4, qbk * 128:(qbk + 1) * 128], add_sink=esk[64:65, g * 512:(g + 1) * 512])
            dma("sp", s_o[:, :, tok0:tok0 + 512], oT[:], ["oT"], [], 3)
        P.barrier()
        A.reset(markB)

    if stage >= 3:
        Wa = A([64, 8, D], BF16, "Wa")
        Wb = A([64, 8, D], BF16, "Wb")
        Wo = A([128, 8, D], BF16, "Wo")
        wr = A([128, 8, NE], F32, "wr")
        dma("pool", Wa[:], wba_d.rearrange("(h p) n -> p h n", p=64), [], ["Wa"], 1)
        dma("pool", Wb[:], wbb_d.rearrange("(h p) n -> p h n", p=64), [], ["Wb"], 1)
        dma("pool", Wo[:], wout_d.rearrange("(k p) n -> p k n", p=128), [], ["Wo"], 1)
        dma("sp", wr[:], wr_d.rearrange("(k p) n -> p k n", p=128), [], ["wr"], 2)
        oc = A([64, 16, 512], BF16)
        sgc = A([128, 16, 512], BF16)
        t1 = A([128, 512], F32)
        t2 = A([128, 512], F32)
        yT = A([128, 8, 512], BF16)
        xt = [A([128, D], F32) for _ in range(2)]
        xnew = [A([128, D], F32) for _ in range(2)]
        tmp = A([128, D], F32)
        junk = A([128, D], F32)
        xn2 = A([128, D], F32)
        ssq = A([128, 1], F32)
        rstd = A([128, 1], F32)
        tmpf = A([128, 8, 128], F32)
        h2f = A([128, 8, 128], F32)
        h2b = [A([128, 8, 128], BF16) for _ in range(2)]
        lg = A([128, NE], F32)
        mx8 = A([128, 8], F32)
        nmx = A([128, 1], F32)
        msk = A([128, NE], F32)
        ee = A([128, NE], F32)
        esum = A([128, 1], F32)
        wts = A([128, NE], F32)
        T32 = PB[:, 5:7, :].rearrange("p a (k t) -> p (a k) t", t=128)
        tctr = 0
        for ch in range(8):
            tok0 = ch * 512
            dma("sp", oc[:], s_o[:, :, tok0:tok0 + 512], [], ["oc"], 2)
            dma("sp", sgc[:], s_gate[:, :, tok0:tok0 + 512], [], ["sgc"], 2)
            for c in range(8):
                for h in range(8):
                    mm(PB[:, 0, :], Wa[:, h, c * 128:(c + 1) * 128], oc[:, h, :], h == 0, h == 7, ["Wa", "oc"], [("pb", 0)])
                for h in range(8):
                    mm(PB[:, 1, :], Wb[:, h, c * 128:(c + 1) * 128], oc[:, 8 + h, :], h == 0, h == 7, ["Wb", "oc"], [("pb", 1)])
                tt("dve", t1[:], PB[:, 0, :], sgc[:, c, :], ALU.mult, [("pb", 0), "sgc"], ["t1"])
                tt("dve", t2[:], PB[:, 1, :], sgc[:, 8 + c, :], ALU.mult, [("pb", 1), "sgc"], ["t2"])
                tt("pool", yT[:, c, :], t1[:], t2[:], ALU.add, ["t1", "t2"], [("yT", c)])
            YT = [("yT", c) for c in range(8)]
            for t in range(4):
                b = tctr % 2
                tctr += 1
                r0 = tok0 + t * 128
                dma("sp", xt[b][:], x_d[r0:r0 + 128, :], [], [("xt", b)], 4)
                for half in range(2):
                    hs = slice(half * 512, (half + 1) * 512)
                    bk = 2 + half
                    for c in range(8):
                        mm(PB[:, bk, :], yT[:, c, t * 128:(t + 1) * 128], Wo[:, c, hs], c == 0, c == 7, YT + ["Wo"], [("pb", bk)])
                    tt("dve", tmp[:, hs], PB[:, bk, :], g1bc[:, hs], ALU.mult, [("pb", bk), ("g1bc", half)], [("tmp", half)])
                    tt("pool", xnew[b][:, hs], tmp[:, hs], xt[b][:, hs], ALU.add, [("tmp", half), ("xt", b)], [("xnew", b, half)])
                XN = [("xnew", b, 0), ("xnew", b, 1)]
                dma("sp", s_xnew[r0:r0 + 128, :], xnew[b][:], XN, [], 3)
                act(junk[:], xnew[b][:], AF.Square, XN, ["junk", "ssq"], accum_out=ssq[:])
                rsqrt(ssq[:], rstd[:], 1024.0, ["ssq"], ["rstd"], "x2")
                act(xn2[:], xnew[b][:], AF.Copy, XN + ["rstd"], ["xn2"], scale=rstd[:, 0:1])
                for k in range(8):
                    tr(T32[:, k, :], xn2[:, k * 128:(k + 1) * 128], identf[:], ["xn2", "identf"], ["T32"])
                tt("dve", tmpf[:], T32, bcast(gs2[:], 2, [128, 8, 128]), ALU.mult, ["T32", "gs2"], ["tmpf"])
                tt("dve", h2f[:], tmpf[:], bcast(sh2[:], 2, [128, 8, 128]), ALU.add, ["tmpf", "sh2"], ["h2f"])
                cp("pool", h2b[b][:], h2f[:], ["h2f"], [("h2b", b)])
                dma("sp", s_h2[:, :, r0:r0 + 128], h2b[b][:], [("h2b", b)], [], 3)
                for k in range(8):
                    mm(PB[:, 4, 0:NE], h2f[:, k, :], wr[:, k, :], k == 0, k == 7, ["h2f", "wr"], [("pb", 4)])
                tt("dve", lg[:], PB[:, 4, 0:NE], rows_t[:, R_BR:R_BR + NE], ALU.add, [("pb", 4), "rows"], ["lg"])
                P.op("dve", lambda e: e.max(out=mx8[:], in_=lg[:]), reads=["lg"], writes=["mx8"])
                ts("dve", msk[:], lg[:], mx8[:, 3:4], None, ALU.is_ge, None, ["lg", "mx8"], ["msk"])
                ts("dve", nmx[:], mx8[:, 0:1], -1.0, None, ALU.mult, None, ["mx8"], ["nmx"])
                act(ee[:], lg[:], AF.Exp, ["lg", "nmx"], ["ee"], bias=nmx[:, 0:1], scale=1.0)
                tt("dve", ee[:], ee[:], msk[:], ALU.mult, ["ee", "msk"], ["ee"])
                P.op("dve", lambda e: e.reduce_sum(out=esum[:], in_=ee[:], axis=AX.X), reads=["ee"], writes=["esum"])
                P.op("dve", lambda e: e.reciprocal(out=esum[:], in_=esum[:]), reads=["esum"], writes=["esum"])
                ts("dve", wts[:], ee[:], esum[:, 0:1], None, ALU.mult, None, ["ee", "esum"], ["wts"])
                tr(PB[0:32, 4, 256:384], wts[:], identf[:], ["wts", "identf"], [("pb", 4)])
                cp("act", WT[:, r0:r0 + 128], PB[0:32, 4, 256:384], [("pb", 4)], [("WT", r0)])
                cp("dve", WTb[:, r0:r0 + 128], PB[0:32, 4, 256:384], [("pb", 4)], [("WTb", r0)])
        if dbg:
            dma("sp", s_wt, WT[:], [("WT", r) for r in range(0, NTOK, 128)], [], 3)
        P.barrier()
        A.reset(markA)

    if stage >= 4:
        wgu = [A([128, 8, 2 * D], BF16) for _ in range(2)]
        wdn = A([128, 8, D], BF16)
        bdn = A([32, D], BF16)
        h2c = A([128, 8, 1024], BF16)
        yacc = A([128, 8, D], F32)
        actT = [A([128, 8, 512], BF16) for _ in range(2)]
        wbc = [A([128, 512], F32) for _ in range(2)]
        selw = [A([32, 128], F32) for _ in range(2)]
        gl = A([128, 512], F32)
        sgm = A([128, 512], F32)
        lb = A([128, 512], F32)
        tA = A([128, 512], F32)
        tB = A([128, 512], F32)
        xo = [A([128, D], F32) for _ in range(1)]
        fo = [A([128, D], F32) for _ in range(2)]
        dma("pool", bdn[:], bdn_d, [], ["bdn"], 1)
        units = [(tc, e, sub) for tc in range(4) for e in range(NE) for sub in range(2)]
        wgu_v = wgu_d.rearrange("e (k p) n -> e p k n", p=128)
        wdn_v = wdn_d.rearrange("e (k p) n -> e p k n", p=128)
        gctr = [0]

        def load_w(tc, e):
            b = e % 2
            for k in range(8):
                for c2 in range(2):
                    dma("pool", wgu[b][:, k, c2 * D:(c2 + 1) * D], wgu_v[e, :, k, c2 * D:(c2 + 1) * D], [], [("wgu", b, k)], 5 if b == 0 else 8)

        def load_dn(e):
            for k2 in range(4):
                dma("pool", wdn[:, 2 * k2:2 * k2 + 2, :], wdn_v[e, :, 2 * k2:2 * k2 + 2, :], [], [("wdn", k2)], 6)

        def GU(ui):
            tc, e, sub = units[ui]
            b = e % 2
            ab = ui % 2
            t0 = tc * 1024 + sub * 512
            hs = slice(sub * 512, (sub + 1) * 512)
            cp("dve", selw[ab][:], identf[0:32, e:e + 1].to_broadcast([32, 128]), ["identf"], [("selw", ab)])
            mm(PB[:, 6, :], selw[ab][:], WT[:, t0:t0 + 512], True, True,
               [("WT", r) for r in range(t0, t0 + 512, 128)] + [("selw", ab)], [("pb", 6)])
            cp("act", wbc[ab][:], PB[:, 6, :], [("pb", 6)], [("wbc", ab)])
            for j in range(8):
                gb = gctr[0] % 2
                gctr[0] += 1
                for k in range(8):
                    mm(PB[:, gb, :], wgu[b][:, k, j * 128:(j + 1) * 128], h2c[:, k, hs], k == 0, k == 7, [("wgu", b, k), "h2c"], [("pb", gb)])
                for k in range(8):
                    mm(PB[:, 2 + gb, :], wgu[b][:, k, D + j * 128:D + (j + 1) * 128], h2c[:, k, hs], k == 0, k == 7,
                       [("wgu", b, k), "h2c"], [("pb", 2 + gb)])
                cg = C_BGU + e * 16 + j
                ts("dve", gl[:], PB[:, gb, :], cols_t[:, cg:cg + 1], 7.0, ALU.add, ALU.min, [("pb", gb), "cols"], ["gl"])
                act(sgm[:], gl[:], AF.Sigmoid, ["gl"], ["sgm"], scale=1.702)
                act(lb[:], PB[:, 2 + gb, :], AF.Identity, [("pb", 2 + gb), "cols"], ["lb"], bias=cols_t[:, cg + 8:cg + 9], scale=1.0)
                ts("dve", lb[:], lb[:], -7.0, 7.0, ALU.max, ALU.min, ["lb"], ["lb"])
                tt("dve", tA[:], gl[:], sgm[:], ALU.mult, ["gl", "sgm"], ["tA"])
                stt("dve", tB[:], lb[:], 1.0, tA[:], ALU.add, ALU.mult, ["lb", "tA"], ["tB"])
                tt("dve", actT[ab][:, j, :], tB[:], wbc[ab][:], ALU.mult, ["tB", ("wbc", ab)], [("actT", ab, j)])

        def DN(ui):
            tc, e, sub = units[ui]
            ab = ui % 2
            AT = [("actT", ab, j) for j in range(8)]
            for t in range(4):
                tg = sub * 4 + t
                r0 = tc * 1024 + tg * 128
                for half in range(2):
                    hs = slice(half * 512, (half + 1) * 512)
                    bk = 4 + half
                    if e == 0:
                        mm(PB[:, bk, :], WTb[:, r0:r0 + 128], bdn[:, hs], True, False, [("WTb", r0), "bdn"], [("pb", bk)])
                    for j in range(8):
                        mm(PB[:, bk, :], actT[ab][:, j, t * 128:(t + 1) * 128], wdn[:, j, hs], (j == 0 and e != 0), j == 7,
                           AT + [("wdn", j // 2)], [("pb", bk)])
                    if e == 0:
                        cp("act", yacc[:, tg, hs], PB[:, bk, :], [("pb", bk)], [("yacc", tg, half)])
                    else:
                        tt("dve", yacc[:, tg, hs], yacc[:, tg, hs], PB[:, bk, :], ALU.add, [("pb", bk), ("yacc", tg, half)], [("yacc", tg, half)])

        def FIN(tc):
            for tg in range(8):
                b = tg % 2
                r0 = tc * 1024 + tg * 128
                dma("sp", xo[0][:], s_xnew[r0:r0 + 128, :], [], [("xo", 0)], 2)
                tt("dve", fo[b][:], yacc[:, tg, :], g2bc[:], ALU.mult, [("yacc", tg, 0), ("yacc", tg, 1), ("g2bc", 0), ("g2bc", 1)], [("fo", b)])
                tt("dve", fo[b][:], fo[b][:], xo[0][:], ALU.add, [("fo", b), ("xo", 0)], [("fo", b)])
                dma("sp", out_d[r0:r0 + 128, :], fo[b][:], [("fo", b)], [], 7)

        nU = len(units)
        for ui in range(nU + 1):
            if ui < nU:
                tc, e, sub = units[ui]
                if sub == 0:
                    if e == 0:
                        dma("sp", h2c[:], s_h2[:, :, tc * 1024:(tc + 1) * 1024], [], ["h2c"], 2)
                    if ui == 0:
                        load_w(tc, 0)
                    ne = ui + 2
                    if ne < nU:
                        load_w(units[ne][0], units[ne][1])
                    if ui == 0:
                        load_dn(0)
                GU(ui)
            if ui >= 1:
                DN(ui - 1)
                ptc, pe_, psub = units[ui - 1]
                if pe_ == NE - 1 and psub == 1:
                    FIN(ptc)
            if ui < nU and ui >= 1 and units[ui][2] == 0:
                load_dn(units[ui][1])
    P.emit()
    return nc, P


def _rope_tables(dim):
    q = dim // 4
    n = NTOK
    row = np.repeat(np.arange(n // 64, dtype=np.int32), 64).astype(np.float32)
    col = np.tile(np.arange(64, dtype=np.int32), n // 64).astype(np.float32)
    freqs = (np.float32(10000.0) ** (-np.arange(q, dtype=np.float32) / np.float32(q))).astype(np.float32)
    ar = row[:, None] * freqs
    ac = col[:, None] * freqs
    ang = np.concatenate([ar, ar, ac, ac], axis=-1).astype(np.float32)
    cos = np.cos(ang).astype(np.float32)
    sin = np.sin(ang).astype(np.float32)
    sgn = np.concatenate([-np.ones(q), np.ones(q), -np.ones(q), np.ones(q)]).astype(np.float32)
    return cos, sin * sgn


def _prep(inp):
    f = lambda a: np.ascontiguousarray(np.asarray(a, dtype=np.float32))
    L = 0
    cm, sm = _rope_tables(32)
    cs, ss = _rope_tables(64)
    rope = f(np.concatenate([cm, sm, cs, ss], axis=1).reshape(32, 128, 192))
    kj = np.arange(128)[:, None]
    qi = np.arange(128)[None, :]
    masks = f(np.concatenate([(qi <= kj), (qi >= kj)], axis=1))
    bgu = np.asarray(inp["b_gate_up"][L], np.float32)
    bgu_cols = np.zeros((128, 512), np.float32)
    for e in range(NE):
        bgu_cols[:, e * 16:e * 16 + 8] = bgu[e, 0::2].reshape(8, 128).T
        bgu_cols[:, e * 16 + 8:e * 16 + 16] = bgu[e, 1::2].reshape(8, 128).T
    wgu = np.asarray(inp["w_gate_up"][L], np.float32)
    wgu_p = f(np.concatenate([wgu[:, :, 0::2], wgu[:, :, 1::2]], axis=2))
    shared = dict(
        rope=rope, masks=masks,
        w_ada=f(inp["w_ada"][L]), w_in=f(inp["w_in"][L]), w_q_up=f(inp["w_q_up"][L]), w_kv_up=f(inp["w_kv_up"][L]),
        w_branch_a=f(inp["w_branch_a"][L]), w_branch_b=f(inp["w_branch_b"][L]), w_out=f(inp["w_out"][L]),
        w_router=f(inp["w_router"][L]), w_gu=wgu_p, w_dn=f(inp["w_down"][L]), b_dn=f(inp["b_down"][L]),
    )
    b_ada = np.asarray(inp["b_ada"][L], np.float32)
    rep = lambda v: np.broadcast_to(np.asarray(v, np.float32)[None, :], (128, len(v)))
    rows = f(np.concatenate([
        rep(inp["mla_q_g"][L]), rep(inp["mla_k_g"][L]), rep(inp["swa_q_g"][L]), rep(inp["swa_k_g"][L]),
        rep(inp["b_router"][L]), rep(np.repeat(np.asarray(inp["swa_sink"][L], np.float32), 128)),
        rep(b_ada[2048:3072]), rep(b_ada[5120:6144])], axis=1))
    assert rows.shape == (128, NROW)
    in_maps = []
    for b in range(8):
        colT = lambda v: np.asarray(v, np.float32).reshape(-1, 128).T
        cols = f(np.concatenate([
            colT(b_ada), colT(inp["norm1_g"][L]), colT(inp["norm2_g"][L]), colT(inp["c"][b]), colT(inp["c_ctx"]),
            colT(inp["mla_q_a_g"][L]), colT(inp["mla_kv_a_g"][L]), bgu_cols], axis=1))
        assert cols.shape == (128, NCOL)
        m = dict(shared)
        m.update(x=f(inp["x"][b]), ctx=f(inp["ctx"][b]), cols=cols, rows=rows)
        in_maps.append(m)
    return in_maps


_CACHE = {}


def kernel(**inputs):
    in_maps = _prep(inputs)
    if "nc" not in _CACHE:
        _CACHE["nc"] = build()[0]
    res = run_bass_kernel_spmd(_CACHE["nc"], in_maps, core_ids=list(range(8)))
    return np.stack([np.asarray(r["out"], dtype=np.float32) for r in res.results], axis=0)
```

```python
import contextlib
import numpy as np
import concourse.bass as bass
import concourse.mybir as mybir
from concourse.bass_utils import run_bass_kernel_spmd

F32 = mybir.dt.float32
BF16 = mybir.dt.bfloat16
AF = mybir.ActivationFunctionType
ALU = mybir.AluOpType
AX = mybir.AxisListType

ENGS = ("pe", "act", "dve", "pool", "sp")
D = 1024
NTOK = 4096
NCTX = 256
NKEY = NTOK + NCTX
NKT = NKEY // 128
NE = 32
EPS = 1e-6

C_BADA, C_G1, C_G2, C_C, C_QAG, C_KVAG, C_BGU = 0, 48, 56, 64, 80, 83, 85
NCOL = 85 + 512
R_QG, R_KG, R_SQG, R_SKG, R_BR, R_SINK, R_G1B, R_G2B = 0, 96, 192, 256, 320, 352, 1376, 2400
NROW = 3424


class Prog:
    def __init__(self, nc, same_engine_sync=True):
        self.nc = nc
        self.ops = []
        self.last_w = {}
        self.readers = {}
        self.same_engine_sync = same_engine_sync

    mute = False
    RING = 8

    def op(self, eng, fn, reads=(), writes=(), stream=None):
        if self.mute:
            return None
        _ps = lambda r: r in ("pmod", "T0", "pm0", "pm1", "pkv", "T32") or (isinstance(r, tuple) and r[0] == "pb")
        writes = list(writes) + [r for r in reads if _ps(r)]
        reads = [r for r in reads if not _ps(r)]
        idx = len(self.ops)
        deps = set()
        for r in reads:
            if r in self.last_w:
                deps.add(self.last_w[r])
        for w in writes:
            if w in self.last_w:
                deps.add(self.last_w[w])
            for rd in self.readers.get(w, ()):
                deps.add(rd)
        for w in writes:
            self.last_w[w] = idx
            self.readers[w] = []
        for r in reads:
            self.readers.setdefault(r, []).append(idx)
        deps.discard(idx)
        self.ops.append(dict(eng=eng, fn=fn, deps=deps, stream=stream, inc=False))
        return idx

    def barrier(self):
        self.mute = False
        last = {}
        cnts = {}
        for i, o in enumerate(self.ops):
            if o["fn"] is None:
                continue
            if o["stream"] is not None:
                c = cnts.get(o["stream"], 0)
                cnts[o["stream"]] = c + 1
                key = ("s", o["stream"], c % self.RING)
            else:
                key = ("e", o["eng"])
            last[key] = i
        deps = set(last.values())
        for e in ENGS:
            self.ops.append(dict(eng=e, fn=None, deps=set(deps), stream=None, inc=False))
        self.last_w = {}
        self.readers = {}

    def _skip(self, od, o):
        return (od["stream"] is None and o["stream"] is None and od["eng"] == o["eng"]
                and (od["eng"] == "pe" or not self.same_engine_sync))

    def emit(self):
        nc = self.nc
        ops = self.ops
        for o in ops:
            for d in o["deps"]:
                od = ops[d]
                if od["stream"] is None and not self._skip(od, o):
                    od["inc"] = True
        cnt = {}
        scnt = {}
        ring_last = {}
        for i, o in enumerate(ops):
            if o["stream"] is not None:
                c = scnt.get(o["stream"], 0)
                scnt[o["stream"]] = c + 1
                k = ("s", o["stream"], c % self.RING)
                cnt[k] = cnt.get(k, 0) + 16
                o["done"] = (k, cnt[k])
                if k in ring_last:
                    o["deps"].add(ring_last[k])
                ring_last[k] = i
            elif o["inc"]:
                k = ("e", o["eng"])
                cnt[k] = cnt.get(k, 0) + 1
                o["done"] = (k, cnt[k])
            else:
                o["done"] = None
        self.final_counts = dict(cnt)
        with contextlib.ExitStack() as st:
            sems = {}
            for k in cnt:
                sems[k] = st.enter_context(nc.semaphore("sem_" + "_".join(str(v) for v in k)))
            block = st.enter_context(nc.Block())

            def run_engine(ename, handle):
                known = {}
                for o in ops:
                    if o["eng"] != ename:
                        continue
                    need = {}
                    for d in o["deps"]:
                        od = ops[d]
                        if od["done"] is None or self._skip(od, o):
                            continue
                        k, v = od["done"]
                        if need.get(k, 0) < v:
                            need[k] = v
                    for k, v in need.items():
                        if known.get(k, 0) < v:
                            handle.wait_ge(sems[k], v)
                            known[k] = v
                    if o["fn"] is None:
                        continue
                    ins = o["fn"](handle)
                    if o["done"] is not None:
                        k, v = o["done"]
                        ins.then_inc(sems[k], 16 if k[0] == "s" else 1)
                if ename == "sp":
                    for k, v in cnt.items():
                        if k[0] == "s" and known.get(k, 0) < v:
                            handle.wait_ge(sems[k], v)

            @block.tensor
            def _(e):
                run_engine("pe", e)

            @block.scalar
            def _(e):
                run_engine("act", e)

            @block.vector
            def _(e):
                run_engine("dve", e)

            @block.gpsimd
            def _(e):
                run_engine("pool", e)

            @block.sync
            def _(e):
                run_engine("sp", e)


class Alloc:
    def __init__(self, nc):
        self.nc = nc
        self.top = (int(nc._sbuf_addr_for_side("left")) + 63) // 64 * 64
        self.limit = int(nc._sbuf_addr_for_side("right")) // 64 * 64
        self.n = 0

    def __call__(self, shape, dt, name=None):
        per = int(np.prod(shape[1:])) * (2 if dt == BF16 else 4)
        per = (per + 63) // 64 * 64
        off = self.top
        self.top += per
        assert self.top <= self.limit, f"SBUF overflow {self.top}"
        self.n += 1
        return self.nc.alloc_sbuf_tensor_at(name or f"t{self.n}_{off}", list(shape), dt, offset=off)

    def mark(self):
        return self.top

    def reset(self, m):
        self.top = m


def bcast(ap, axis, shape):
    return ap.unsqueeze(axis).to_broadcast(list(shape))


def build(stage=99, dbg=False):
    nc = bass.Bass("TRN2", target_bir_lowering=False)
    P = Prog(nc)

    def din(name, shape, dt=F32):
        return nc.dram_tensor(name, list(shape), dt, kind="ExternalInput").ap()

    def dscr(name, shape, dt):
        return nc.dram_tensor(name, list(shape), dt, kind=("ExternalOutput" if dbg else "Internal")).ap()

    x_d = din("x", [NTOK, D])
    ctx_d = din("ctx", [NCTX, D])
    cols_d = din("cols", [128, NCOL])
    rows_d = din("rows", [128, NROW])
    rope_d = din("rope", [32, 128, 192])
    mask_d = din("masks", [128, 256])
    wada_d = din("w_ada", [D, 6 * D])
    win_d = din("w_in", [D, 3488])
    wqup_d = din("w_q_up", [384, 768])
    wkvup_d = din("w_kv_up", [256, 1024])
    wba_d = din("w_branch_a", [512, D])
    wbb_d = din("w_branch_b", [512, D])
    wout_d = din("w_out", [D, D])
    wr_d = din("w_router", [D, NE])
    wgu_d = din("w_gu", [NE, D, 2 * D])
    wdn_d = din("w_dn", [NE, D, D])
    bdn_d = din("b_dn", [NE, D])
    out_d = nc.dram_tensor("out", [NTOK, D], F32, kind="ExternalOutput").ap()

    s_kt = dscr("s_kt", [96, 8, NKEY], BF16)
    s_va = dscr("s_va", [8, 128, NKT, 65], BF16)
    s_kst = dscr("s_kst", [128, NKEY], BF16)
    s_vsa = dscr("s_vsa", [128, NKT, 130], BF16)
    s_qm = dscr("s_qm", [96, 8, NTOK], BF16)
    s_qs = dscr("s_qs", [128, 32, 512], BF16)
    s_gate = dscr("s_gate", [128, 16, NTOK], BF16)
    s_o = dscr("s_o", [64, 16, NTOK], BF16)
    s_xnew = dscr("s_xnew", [NTOK, D], F32)
    s_h2 = dscr("s_h2", [128, 8, NTOK], BF16)
    if dbg:
        s_wt = dscr("s_wt", [32, NTOK], F32)

    A = Alloc(nc)
    T0t = nc.alloc_psum_tensor("t0", [128, 8, 128], BF16)
    T0b = T0t[:, :, :]
    PB = nc.alloc_psum_tensor("pb", [128, 7, 512], F32)

    def bank2(i):
        return PB[:, i:i + 2, :].rearrange("p a b -> p (a b)")

    def dma(eng, out, in_, reads, writes, stream):
        P.op(eng, lambda e: e.dma_start(out=out, in_=in_), reads=reads, writes=writes, stream=stream)

    def mm(out, lhsT, rhs, start, stop, reads, writes):
        P.op("pe", lambda e: e.matmul(out, lhsT=lhsT, rhs=rhs, start=start, stop=stop), reads=reads, writes=writes)

    def tr(out, in_, ident, reads, writes):
        P.op("pe", lambda e: e.transpose(out=out, in_=in_, identity=ident), reads=reads, writes=writes)

    def act(out, in_, func, reads, writes, **kw):
        P.op("act", lambda e: e.activation(out=out, in_=in_, func=func, **kw), reads=reads, writes=writes)

    def tt(eng, out, in0, in1, op, reads, writes):
        P.op(eng, lambda e: e.tensor_tensor(out=out, in0=in0, in1=in1, op=op), reads=reads, writes=writes)

    def ts(eng, out, in0, s1, s2, op0, op1, reads, writes):
        if s2 is None:
            P.op(eng, lambda e: e.tensor_scalar(out=out, in0=in0, scalar1=s1, scalar2=None, op0=op0), reads=reads, writes=writes)
        else:
            P.op(eng, lambda e: e.tensor_scalar(out=out, in0=in0, scalar1=s1, scalar2=s2, op0=op0, op1=op1), reads=reads, writes=writes)

    def stt(eng, out, in0, scalar, in1, op0, op1, reads, writes):
        P.op(eng, lambda e: e.scalar_tensor_tensor(out=out, in0=in0, scalar=scalar, in1=in1, op0=op0, op1=op1),
             reads=reads, writes=writes)

    def cp(eng, out, in_, reads, writes):
        if eng == "act":
            P.op("act", lambda e: e.copy(out=out, in_=in_), reads=reads, writes=writes)
        else:
            P.op(eng, lambda e: e.tensor_copy(out=out, in_=in_), reads=reads, writes=writes)

    def rsqrt(src, dst, n, reads, writes, tag):
        act(dst, src, AF.Sqrt, reads, writes, scale=1.0 / n, bias=EPS)
        P.op("dve", lambda e: e.reciprocal(out=dst, in_=dst), reads=writes, writes=writes)

    cols_t = A([128, NCOL], F32, "cols_t")
    g2bc = A([128, D], F32, "g2bc")
    WT = A([32, NTOK], F32, "WT")
    WTb = A([32, NTOK], BF16, "WTb")
    identf = A([128, 128], F32, "identf")
    ident = A([128, 128], BF16, "ident")
    eps_t = A([128, 1], F32, "eps_t")
    gs2 = A([128, 8], F32, "gs2")
    sh2 = A([128, 8], F32, "sh2")
    markA = A.mark()
    rows_t = A([128, NROW], F32, "rows_t")
    g1bc = A([128, D], F32, "g1bc")
    modT = A([128, 48, 2], F32, "modT")
    gs1 = A([128, 8, 2], F32, "gs1")
    onesb = A([128, 128], BF16, "onesb")
    onesf = A([128, 64], F32, "onesf")
    maskb = A([128, 256], BF16, "maskb")
    markB = A.mark()

    dma("sp", cols_t[:], cols_d, [], ["cols"], 0)
    dma("sp", rows_t[:], rows_d, [], ["rows"], 0)
    maskf = A([128, 256], F32)
    dma("sp", maskf[:], mask_d, [], ["maskf"], 0)
    P.op("pool", lambda e: e.memset(identf[:], 0.0), writes=["identf"])
    P.op("pool", lambda e: e.affine_select(out=identf[:], in_=identf[:], pattern=[[-1, 128]], compare_op=ALU.not_equal,
                                            fill=1.0, base=0, channel_multiplier=1), reads=["identf"], writes=["identf"])
    cp("dve", ident[:], identf[:], ["identf"], ["ident"])
    cp("dve", maskb[:], maskf[:], ["maskf"], ["maskb"])
    P.op("pool", lambda e: e.memset(onesb[:], 1.0), writes=["onesb"])
    P.op("pool", lambda e: e.memset(onesf[:], 1.0), writes=["onesf"])
    P.op("pool", lambda e: e.memset(eps_t[:], EPS), writes=["eps"])
    sc = A([128, 8, 2], F32)
    scB = A([128, 8, 128], F32)
    act(sc[:, :, 0], cols_t[:, C_C:C_C + 8], AF.Silu, ["cols"], ["sc0"])
    act(sc[:, :, 1], cols_t[:, C_C + 8:C_C + 16], AF.Silu, ["cols"], ["sc1"])
    cp("dve", scB[:], sc[:, :, 0:1].to_broadcast([128, 8, 128]), ["sc0"], ["scB"])
    wa = [A([128, 8, 512], F32) for _ in range(2)]
    wada_v = wada_d.rearrange("(k p) n -> p k n", p=128)
    pmod = PB[:, 0, 0:96].rearrange("p (j s) -> p j s", s=2)
    bcbank = {4: 1, 5: 2, 10: 3, 11: 4}
    for cc in range(12):
        b = cc % 2
        dma("sp", wa[b][:], wada_v[:, :, cc * 512:(cc + 1) * 512], [], [("wa", b)], 0)
        for jj in range(4):
            j = cc * 4 + jj
            for k in range(8):
                mm(pmod[:, j, :], wa[b][:, k, jj * 128:(jj + 1) * 128], sc[:, k, :], k == 0, k == 7,
                   [("wa", b), "sc0", "sc1"], ["pmod"])
        if cc in bcbank:
            bk = bcbank[cc]
            for k in range(8):
                mm(PB[:, bk, :], scB[:, k, :], wa[b][:, k, :], k == 0, k == 7, [("wa", b), "scB"], [("pb", bk)])
            half = cc % 2
            if cc < 6:
                tt("dve", g1bc[:, half * 512:(half + 1) * 512], PB[:, bk, :], rows_t[:, R_G1B + half * 512:R_G1B + (half + 1) * 512],
                   ALU.add, [("pb", bk), "rows"], [("g1bc", half)])
            else:
                tt("dve", g2bc[:, half * 512:(half + 1) * 512], PB[:, bk, :], rows_t[:, R_G2B + half * 512:R_G2B + (half + 1) * 512],
                   ALU.add, [("pb", bk), "rows"], [("g2bc", half)])
    tt("dve", modT[:], pmod, bcast(cols_t[:, C_BADA:C_BADA + 48], 2, [128, 48, 2]), ALU.add, ["pmod", "cols"], ["modT"])
    stt("dve", gs1[:], modT[:, 8:16, :], 1.0, bcast(cols_t[:, C_G1:C_G1 + 8], 2, [128, 8, 2]), ALU.add, ALU.mult,
        ["modT", "cols"], ["gs1"])
    stt("dve", gs2[:], modT[:, 32:40, 0], 1.0, cols_t[:, C_G2:C_G2 + 8], ALU.add, ALU.mult, ["modT", "cols"], ["gs2"])
    cp("dve", sh2[:], modT[:, 24:32, 0], ["modT"], ["sh2"])
    P.barrier()
    A.reset(markB)

    if stage >= 1:
        win = A([128, 8, 3488], BF16, "win")
        wkv = A([128, 2, 1024], BF16, "wkv")
        wq = A([128, 3, 768], BF16, "wq")
        win_v = win_d.rearrange("(k p) n -> p k n", p=128)
        import os as _os
        SKIP = _os.environ.get('K_SKIP', '')
        if 'w' in SKIP:
            P.mute = True
        for k in range(8):
            for c4 in range(4):
                dma("pool", win[:, k, c4 * 872:(c4 + 1) * 872], win_v[:, k, c4 * 872:(c4 + 1) * 872], [], [("win", k)], 1)
        dma("pool", wkv[:], wkvup_d.rearrange("(k p) n -> p k n", p=128), [], ["wkv"], 1)
        dma("pool", wq[:], wqup_d.rearrange("(k p) n -> p k n", p=128), [], ["wq"], 1)
        WIN = [("win", k) for k in range(8)]
        P.mute = False
        xt = [A([128, D], F32) for _ in range(2)]
        junk = A([128, D], F32)
        ssq = A([128, 1], F32)
        rstd = A([128, 1], F32)
        xn = A([128, D], BF16)
        tmpf = A([128, 8, 128], F32)
        hT = [A([128, 8, 512], BF16) for _ in range(1)]
        latf = A([128, 5, 512], F32)
        sq = A([128, 5, 512], BF16)
        rbc = A([128, 512], F32)
        latn = A([128, 5, 512], BF16)
        sg = A([128, 8, 512], BF16)
        ropet = [A([128, 192], F32) for _ in range(2)]
        scr = A([128, 768], F32)
        scr2 = A([128, 768], F32)
        rtA = A([128, 512], F32)
        rtB = A([128, 512], F32)
        st8 = A([128, 8], F32)
        ssqr = A([128, 1], F32)
        kn = A([128, 8, 96], BF16)
        krg = A([128, 32], F32)
        krr = A([128, 32], F32)
        ktile = A([128, 8, 128], BF16)
        vtile = [A([128, 8, 65], BF16) for _ in range(2)]
        qb = A([128, 8, 96], BF16)
        qtile = A([128, 8, 128], BF16)
        ksb = A([128, 128], BF16)
        kstile = A([128, 128], BF16)
        vstile = [A([128, 2, 65], BF16) for _ in range(2)]
        qsb = A([128, 8, 64], BF16)
        qsbP = A([128, 4, 2, 64], BF16)
        qstile = A([128, 4, 128], BF16)
        for b in range(0 if 'm' not in SKIP else 2, 2):
            P.op("pool", lambda e, b=b: e.memset(vtile[b][:], 1.0), writes=[("vtile", b)])
            P.op("pool", lambda e, b=b: e.memset(vstile[b][:], 1.0), writes=[("vstile", b)])

        def rope(src3, H, R, cos, ss, dst3, rd, wr):
            q = R // 4
            a3 = rtA[:, 0:H * R].rearrange("p (h r) -> p h r", h=H)
            b3 = rtB[:, 0:H * R].rearrange("p (h r) -> p h r", h=H)
            tt("pool", a3, src3, bcast(cos, 1, [128, H, R]), ALU.mult, rd, ["rtA"])
            s5 = src3.rearrange("p h (a b c) -> p h a b c", a=2, b=2)
            b5 = b3.rearrange("p h (a b c) -> p h a b c", a=2, b=2)
            ss4 = ss.rearrange("p (a b c) -> p a b c", a=2, b=2)
            tt("pool", b5[:, :, :, 0, :], s5[:, :, :, 1, :], bcast(ss4[:, :, 0, :], 1, [128, H, 2, q]), ALU.mult, rd, ["rtB0"])
            tt("pool", b5[:, :, :, 1, :], s5[:, :, :, 0, :], bcast(ss4[:, :, 1, :], 1, [128, H, 2, q]), ALU.mult, rd, ["rtB1"])
            tt("pool", dst3, a3, b3, ALU.add, ["rtA", "rtB0", "rtB1"], wr)

        def headnorm(src3, H, Dh, gain, dstf3, rd, tag, extra=None):
            s3 = scr[:, 0:H * Dh].rearrange("p (h d) -> p h d", h=H)
            act(s3, src3, AF.Square, rd, ["scr"])
            P.op("dve", lambda e: e.reduce_sum(out=st8[:, 0:H], in_=s3, axis=AX.X), reads=["scr"], writes=["st8"])
            if extra is not None:
                ts("dve", st8[:, 0:H], st8[:, 0:H], extra[0], None, ALU.add, None, ["st8", extra[1]], ["st8"])
            rsqrt(st8[:, 0:H], st8[:, 0:H], float(Dh if extra is None else 96), ["st8"], ["st8"], tag)
            tt("dve", s3, src3, bcast(st8[:, 0:H], 2, [128, H, Dh]), ALU.mult, rd + ["st8", "scr"], ["scr"])
            tt("pool", dstf3, s3, bcast(gain, 1, [128, H, Dh]), ALU.mult, ["scr", "rows"], [tag])

        supers = [(ctx_d, 0, 2, 1, False, 0)] + [(x_d, i * 512, 4, 0, True, 2 + 4 * i) for i in range(8)]
        import os as _os
        supers = supers[:int(_os.environ.get('K_NSUP', '9'))]
        CUT = int(_os.environ.get('K_CUT', '99'))
        tile_ctr = 0
        for si, (src, row0, NT, sidx, latent, kt0) in enumerate(supers):
            NTK = NT * 128
            hb = 0
            for t in range(NT):
                b = tile_ctr % 2
                tile_ctr += 1
                dma("sp", xt[b][:], src[row0 + t * 128:row0 + (t + 1) * 128, :], [], [("xt", b)], 2)
                act(junk[:], xt[b][:], AF.Square, [("xt", b)], ["junk", "ssq"], accum_out=ssq[:])
                rsqrt(ssq[:], rstd[:], 1024.0, ["ssq"], ["rstd"], "x")
                act(xn[:], xt[b][:], AF.Copy, [("xt", b), "rstd"], ["xn"], scale=rstd[:, 0:1])
                for k in range(8):
                    tr(T0b[:, k, :], xn[:, k * 128:(k + 1) * 128], ident[:], ["xn", "ident"], ["T0"])
                tt("dve", tmpf[:], T0b, bcast(gs1[:, :, sidx], 2, [128, 8, 128]), ALU.mult, ["T0", "gs1"], ["tmpf"])
                tt("dve", hT[hb][:, :, t * 128:(t + 1) * 128], tmpf[:], bcast(modT[:, 0:8, sidx], 2, [128, 8, 128]), ALU.add,
                   ["tmpf", "modT"], [("hT", hb, t)])
            HT = [("hT", hb, t) for t in range(NT)]
            if CUT == 1:
                P.mute = True
            chunks = [(0, 0), (1, 128)] + ([(2, 544), (3, 672), (4, 800)] if latent else [])
            fb = 0
            for ci, c0 in chunks:
                bk = 1 + (fb % 2)
                fb += 1
                for k in range(8):
                    mm(PB[:, bk, 0:NTK], win[:, k, c0:c0 + 128], hT[hb][:, k, 0:NTK], k == 0, k == 7, HT + [("win", k)], [("pb", bk)])
                cp("dve", latf[:, ci, 0:NTK], PB[:, bk, 0:NTK], [("pb", bk)], [("latf", ci)])
                act(sq[:, ci, 0:NTK], PB[:, bk, 0:NTK], AF.Square, [("pb", bk)], [("sq", ci)])
            for (cis, n, goff) in ([([0, 1], 256.0, C_KVAG)] + ([([2, 3, 4], 384.0, C_QAG - 2)] if latent else [])):
                bk = 1 + (fb % 2)
                fb += 1
                for i, ci in enumerate(cis):
                    mm(PB[:, bk, 0:NTK], onesb[:], sq[:, ci, 0:NTK], i == 0, i == len(cis) - 1, [("sq", ci), "onesb"], [("pb", bk)])
                rsqrt(PB[:, bk, 0:NTK], rbc[:, 0:NTK], n, [("pb", bk)], ["rbc"], "lat")
                for ci in cis:
                    stt("dve", latn[:, ci, 0:NTK], latf[:, ci, 0:NTK], cols_t[:, goff + ci:goff + ci + 1], rbc[:, 0:NTK],
                        ALU.mult, ALU.mult, [("latf", ci), "rbc", "cols"], [("latn", ci)])
            if latent:
                for gc in range(16):
                    bk = 1 + (fb % 2)
                    fb += 1
                    c0 = 1440 + gc * 128
                    for k in range(8):
                        mm(PB[:, bk, :], win[:, k, c0:c0 + 128], hT[hb][:, k, :], k == 0, k == 7, HT + [("win", k)], [("pb", bk)])
                    act(sg[:, gc % 8, :], PB[:, bk, :], AF.Sigmoid, [("pb", bk)], [("sg", gc % 8)])
                    if gc % 8 == 7:
                        g0 = gc - 7
                        dma("sp", s_gate[:, g0:g0 + 8, row0:row0 + 512], sg[:], [("sg", i) for i in range(8)], [("sgall")], 3)
            if CUT == 2:
                P.mute = True
            pm = bank2(3)
            pkv = bank2(5)
            for t in range(NT):
                tsl = slice(t * 128, (t + 1) * 128)
                kt = kt0 + t
                key0 = kt * 128
                tok0 = row0 + t * 128
                vb = kt % 2
                if latent:
                    rb = kt % 2
                    dma("sp", ropet[rb][:], rope_d[kt - 2], [], [("rope", rb)], 2)
                    cos_m, ss_m = ropet[rb][:, 0:32], ropet[rb][:, 32:64]
                    cos_s, ss_s = ropet[rb][:, 64:128], ropet[rb][:, 128:192]
                    RP = [("rope", rb)]
                for k in range(8):
                    mm(pm[:, 0:288], hT[hb][:, k, tsl], win[:, k, 256:544], k == 0, k == 7, [("hT", hb, t), ("win", k)], ["pm0"])
                if latent:
                    for k in range(8):
                        mm(pm[:, 512:1024], hT[hb][:, k, tsl], win[:, k, 928:1440], k == 0, k == 7, [("hT", hb, t), ("win", k)], ["pm1"])
                for half in range(2):
                    for c in range(2):
                        mm(pkv[:, half * 512:(half + 1) * 512], latn[:, c, tsl], wkv[:, c, half * 512:(half + 1) * 512], c == 0, c == 1,
                           [("latn", c), "wkv"], ["pkv"])
                pkv3 = pkv.rearrange("p (h d) -> p h d", h=8)
                if CUT == 3:
                    P.mute = True
                act(junk[:, 0:32], pm[:, 0:32], AF.Square, ["pm0"], ["junk", "ssqr"], accum_out=ssqr[:])
                s3 = scr[:, 0:512].rearrange("p (h d) -> p h d", h=8)
                act(s3, pkv3[:, :, 0:64], AF.Square, ["pkv"], ["scr"])
                P.op("dve", lambda e, s3=s3: e.reduce_sum(out=st8[:], in_=s3, axis=AX.X), reads=["scr"], writes=["st8"])
                ts("dve", st8[:], st8[:], ssqr[:, 0:1], None, ALU.add, None, ["st8", "ssqr"], ["st8"])
                rsqrt(st8[:], st8[:], 96.0, ["st8"], ["st8"], "k")
                tt("dve", s3, pkv3[:, :, 0:64], bcast(st8[:], 2, [128, 8, 64]), ALU.mult, ["pkv", "st8", "scr"], ["scr"])
                tt("pool", kn[:, :, 0:64], s3, bcast(rows_t[:, R_KG:R_KG + 64], 1, [128, 8, 64]), ALU.mult, ["scr", "rows"], ["kn0"])
                tt("dve", krg[:], pm[:, 0:32], rows_t[:, R_KG + 64:R_KG + 96], ALU.mult, ["pm0", "rows"], ["krg"])
                if latent:
                    rope(krg[:].unsqueeze(1), 1, 32, cos_m, ss_m, krr[:].unsqueeze(1), ["krg"] + RP, ["krr"])
                    krsrc, KR = krr, "krr"
                else:
                    krsrc, KR = krg, "krg"
                tt("dve", kn[:, :, 64:96], bcast(krsrc[:], 1, [128, 8, 32]), bcast(st8[:], 2, [128, 8, 32]), ALU.mult, [KR, "st8"], ["kn1"])
                for h in range(8):
                    tr(T0b[0:96, h, :], kn[:, h, :], ident[:], ["kn0", "kn1", "ident"], ["T0"])
                cp("act", ktile[0:96, :, :], T0b[0:96, :, :], ["T0"], ["ktile"])
                dma("sp", s_kt[:, :, key0:key0 + 128], ktile[0:96, :, :], ["ktile"], [], 3)
                cp("act", vtile[vb][:, :, 0:64], pkv3[:, :, 64:128], ["pkv"], [("vtile", vb)])
                dma("sp", s_va.rearrange("h p t e -> p h t e")[:, :, kt, :], vtile[vb][:], [("vtile", vb)], [], 3)
                if CUT == 4:
                    P.mute = True
                pks = pm[:, 32:160].rearrange("p (h d) -> p h d", h=2)
                ksn = scr2[:, 0:128].rearrange("p (h d) -> p h d", h=2)
                headnorm(pks, 2, 64, rows_t[:, R_SKG:R_SKG + 64], ksn, ["pm0"], "scr2")
                ksb3 = ksb[:].rearrange("p (h d) -> p h d", h=2)
                if latent:
                    rope(ksn, 2, 64, cos_s, ss_s, ksb3, ["scr2"] + RP, ["ksb"])
                else:
                    cp("pool", ksb3, ksn, ["scr2"], ["ksb"])
                tr(T0b[:, 0, :], ksb[:], ident[:], ["ksb", "ident"], ["T0"])
                cp("act", kstile[:], T0b[:, 0, :], ["T0"], ["kstile"])
                dma("sp", s_kst[:, key0:key0 + 128], kstile[:], ["kstile"], [], 3)
                cp("act", vstile[vb][:, :, 0:64], pm[:, 160:288].rearrange("p (h d) -> p h d", h=2), ["pm0"], [("vstile", vb)])
                dma("sp", s_vsa[:, kt, :], vstile[vb][:].rearrange("p h e -> p (h e)"), [("vstile", vb)], [], 3)
                if not latent:
                    continue
                pq = pkv
                for (c0, c1) in ((0, 512), (512, 768)):
                    for c in range(3):
                        mm(pq[:, c0:c1], latn[:, 2 + c, tsl], wq[:, c, c0:c1], c == 0, c == 2, [("latn", 2 + c), "wq"], ["pkv"])
                pq3 = pq[:, 0:768].rearrange("p (h d) -> p h d", h=8)
                qn3 = scr2[:, 0:768].rearrange("p (h d) -> p h d", h=8)
                headnorm(pq3, 8, 96, rows_t[:, R_QG:R_QG + 96], qn3, ["pkv"], "scr2")
                cp("pool", qb[:, :, 0:64], qn3[:, :, 0:64], ["scr2"], ["qb0"])
                rope(qn3[:, :, 64:96], 8, 32, cos_m, ss_m, qb[:, :, 64:96], ["scr2"] + RP, ["qb1"])
                for h in range(8):
                    tr(T0b[0:96, h, :], qb[:, h, :], ident[:], ["qb0", "qb1", "ident"], ["T0"])
                cp("act", qtile[0:96, :, :], T0b[0:96, :, :], ["T0"], ["qtile"])
                dma("sp", s_qm[:, :, tok0:tok0 + 128], qtile[0:96, :, :], ["qtile"], [], 3)
                pqs = pm[:, 512:1024].rearrange("p (h d) -> p h d", h=8)
                qsn = scr2[:, 0:512].rearrange("p (h d) -> p h d", h=8)
                headnorm(pqs, 8, 64, rows_t[:, R_SQG:R_SQG + 64], qsn, ["pm1"], "scr2")
                rope(qsn, 8, 64, cos_s, ss_s, qsb[:], ["scr2"] + RP, ["qsb"])
                cp("pool", qsbP[:].rearrange("p hg g d -> p g hg d"), qsb[:].rearrange("p (g hg) d -> p g hg d", g=2), ["qsb"], ["qsbP"])
                for hg in range(4):
                    tr(T0b[:, hg, :], qsbP[:, hg, :, :].rearrange("p g d -> p (g d)"), ident[:], ["qsbP", "ident"], ["T0"])
                cp("act", qstile[:], T0b[:, 0:4, :], ["T0"], ["qstile"])
                dma("sp", s_qs[:, kt - 2, :], qstile[:].rearrange("p h q -> p (h q)"), ["qstile"], [], 3)
        P.barrier()
        A.reset(markB)

    if stage >= 2:
        kst = A([128, NKEY], BF16, "kst")
        vsa = A([128, NKT, 130], BF16, "vsa")
        esk = A([128, 1024], F32, "esk")
        dma("sp", kst[:], s_kst, [], ["kst"], 2)
        dma("sp", vsa[:], s_vsa, [], ["vsa"], 2)
        act(esk[:], rows_t[:, R_SINK:R_SINK + 1024], AF.Exp, ["rows"], ["esk"])
        kth = [A([128, NKEY], BF16) for _ in range(2)]
        vah = [A([128, NKT, 65], BF16) for _ in range(2)]
        qm = A([128, 8, 512], BF16)
        qs = A([128, 4, 512], BF16)
        NS = 5
        PT = [A([128, 512], BF16) for _ in range(NS)]
        oT = A([64, 16, 512], BF16)
        osb = A([64, 512], F32)
        rden = A([128, 512], F32)
        sctr = [0]
        octr = [0]
        hctr = 0

        def attend(tiles, qk_fn, pv_fn, masks, tag):
            n = len(tiles)
            ob = 5
            slots = []

            def issue_qk(i):
                sbk = sctr[0] % NS
                sctr[0] += 1
                qk_fn(tiles[i], PB[:, sbk, :], ("pb", sbk))
                slots.append(sbk)

            for i0 in range(min(n, NS - 1)):
                issue_qk(i0)
            for i in range(n):
                sbk = slots[i]
                act(PT[sbk][:], PB[:, sbk, :], AF.Exp, [("pb", sbk)], [("PT", sbk)], scale=tag)
                if masks[i] is not None:
                    m0 = masks[i]
                    p3 = PT[sbk][:].rearrange("p (h q) -> p h q", h=4)
                    tt("pool", p3, p3, bcast(maskb[:, m0:m0 + 128], 1, [128, 4, 128]), ALU.mult, [("PT", sbk), "maskb"], [("PT", sbk)])
                pv_fn(tiles[i], PB[0:65, ob, :], PT[sbk][:], i == 0, i == n - 1, ("PT", sbk), ("pb", ob))
                if i + NS - 1 < n:
                    issue_qk(i + NS - 1)
            return ob

        def finish(ob, dst, add_sink=None):
            if add_sink is not None:
                tt("dve", rden[64:65, :], PB[64:65, ob, :], add_sink, ALU.add, [("pb", ob), "esk"], ["rden"])
                P.op("dve", lambda e: e.reciprocal(out=rden[64:65, :], in_=rden[64:65, :]), reads=["rden"], writes=["rden"])
            else:
                P.op("dve", lambda e: e.reciprocal(out=rden[64:65, :], in_=PB[64:65, ob, :]), reads=[("pb", ob)], writes=["rden"])
            mm(PB[0:64, 6, :], onesf[64:65, 0:64], rden[64:65, :], True, True, ["rden", "onesf"], [("pb", 6)])
            cp("act", osb[:], PB[0:64, ob, :], [("pb", ob)], ["osb"])
            tt("dve", dst, osb[:] if len(dst.shape) == 2 else osb[:].rearrange("p (h q) -> p h q", h=4),
               PB[0:64, 6, :] if len(dst.shape) == 2 else PB[0:64, 6, :].rearrange("p (h q) -> p h q", h=4),
               ALU.mult, ["osb", ("pb", 6)], ["oT"])

        for ch in range(8):
            tok0 = ch * 512
            dma("sp", qm[0:96, :, :], s_qm[:, :, tok0:tok0 + 512], [], ["qm"], 2)
            dma("sp", qs[:], s_qs[:, ch * 4:(ch + 1) * 4, :], [], ["qs"], 2)
            for h in range(8):
                kb = hctr % 2
                hctr += 1
                dma("sp", kth[kb][0:96, :], s_kt[:, h, :], [], [("kth", kb)], 2)
                dma("sp", vah[kb][:], s_va[h], [], [("vah", kb)], 2)

                def qk(kt, S, sres, h=h, kb=kb):
                    mm(S, kth[kb][0:96, kt * 128:(kt + 1) * 128], qm[0:96, h, :], True, True, [("kth", kb), "qm"], [sres])

                def pv(kt, O, Pt, st, sp_, pres, ores, kb=kb):
                    mm(O, vah[kb][:, kt, :], Pt, st, sp_, [("vah", kb), pres], [ores])

                ob = attend(list(range(NKT)), qk, pv, [None] * NKT, 96 ** -0.5)
                finish(ob, oT[:, h, :])
            for g in range(2):
                for qbk in range(4):
                    blk = ch * 4 + qbk
                    tiles, masks = [0, 1], [None, None]
                    for kbk, mk in ((blk - 1, 0), (blk, None), (blk + 1, 128)):
                        if 0 <= kbk < 32:
                            tiles.append(2 + kbk)
                            masks.append(mk)
                    gs_ = slice(g * 64, (g + 1) * 64)

                    def qk(kt, S, sres, gs_=gs_, qbk=qbk):
                        mm(S, kst[gs_, kt * 128:(kt + 1) * 128], qs[gs_, qbk, :], True, True, ["kst", "qs"], [sres])

                    def pv(kt, O, Pt, st, sp_, pres, ores, g=g):
                        mm(O, vsa[:, kt, g * 65:(g + 1) * 65], Pt, st, sp_, ["vsa", pres], [ores])

                    ob = attend(tiles, qk, pv, masks, 0.125)
                    finish(ob, oT[:, 8 + g * 4:8 + g * 4 + 4, qbk * 128:(qbk + 1) * 128], add_sink=esk[64:65, g * 512:(g + 1) * 512])
            dma("sp", s_o[:, :, tok0:tok0 + 512], oT[:], ["oT"], [], 3)
        P.barrier()
        A.reset(markB)

    if stage >= 3:
        Wa = A([64, 8, D], BF16, "Wa")
        Wb = A([64, 8, D], BF16, "Wb")
        Wo = A([128, 8, D], BF16, "Wo")
        wr = A([128, 8, NE], F32, "wr")
        dma("pool", Wa[:], wba_d.rearrange("(h p) n -> p h n", p=64), [], ["Wa"], 1)
        dma("pool", Wb[:], wbb_d.rearrange("(h p) n -> p h n", p=64), [], ["Wb"], 1)
        dma("pool", Wo[:], wout_d.rearrange("(k p) n -> p k n", p=128), [], ["Wo"], 1)
        dma("sp", wr[:], wr_d.rearrange("(k p) n -> p k n", p=128), [], ["wr"], 2)
        oc = A([64, 16, 512], BF16)
        sgc = A([128, 16, 512], BF16)
        t1 = A([128, 512], F32)
        t2 = A([128, 512], F32)
        yT = A([128, 8, 512], BF16)
        xt = [A([128, D], F32) for _ in range(2)]
        xnew = [A([128, D], F32) for _ in range(2)]
        tmp = A([128, D], F32)
        junk = A([128, D], F32)
        xn2 = A([128, D], F32)
        ssq = A([128, 1], F32)
        rstd = A([128, 1], F32)
        tmpf = A([128, 8, 128], F32)
        h2f = A([128, 8, 128], F32)
        h2b = [A([128, 8, 128], BF16) for _ in range(2)]
        lg = A([128, NE], F32)
        mx8 = A([128, 8], F32)
        nmx = A([128, 1], F32)
        msk = A([128, NE], F32)
        ee = A([128, NE], F32)
        esum = A([128, 1], F32)
        wts = A([128, NE], F32)
        T32 = PB[:, 5:7, :].rearrange("p a (k t) -> p (a k) t", t=128)
        tctr = 0
        for ch in range(8):
            tok0 = ch * 512
            dma("sp", oc[:], s_o[:, :, tok0:tok0 + 512], [], ["oc"], 2)
            dma("sp", sgc[:], s_gate[:, :, tok0:tok0 + 512], [], ["sgc"], 2)
            for c in range(8):
                for h in range(8):
                    mm(PB[:, 0, :], Wa[:, h, c * 128:(c + 1) * 128], oc[:, h, :], h == 0, h == 7, ["Wa", "oc"], [("pb", 0)])
                for h in range(8):
                    mm(PB[:, 1, :], Wb[:, h, c * 128:(c + 1) * 128], oc[:, 8 + h, :], h == 0, h == 7, ["Wb", "oc"], [("pb", 1)])
                tt("dve", t1[:], PB[:, 0, :], sgc[:, c, :], ALU.mult, [("pb", 0), "sgc"], ["t1"])
                tt("dve", t2[:], PB[:, 1, :], sgc[:, 8 + c, :], ALU.mult, [("pb", 1), "sgc"], ["t2"])
                tt("pool", yT[:, c, :], t1[:], t2[:], ALU.add, ["t1", "t2"], [("yT", c)])
            YT = [("yT", c) for c in range(8)]
            for t in range(4):
                b = tctr % 2
                tctr += 1
                r0 = tok0 + t * 128
                dma("sp", xt[b][:], x_d[r0:r0 + 128, :], [], [("xt", b)], 4)
                for half in range(2):
                    hs = slice(half * 512, (half + 1) * 512)
                    bk = 2 + half
                    for c in range(8):
                        mm(PB[:, bk, :], yT[:, c, t * 128:(t + 1) * 128], Wo[:, c, hs], c == 0, c == 7, YT + ["Wo"], [("pb", bk)])
                    tt("dve", tmp[:, hs], PB[:, bk, :], g1bc[:, hs], ALU.mult, [("pb", bk), ("g1bc", half)], [("tmp", half)])
                    tt("pool", xnew[b][:, hs], tmp[:, hs], xt[b][:, hs], ALU.add, [("tmp", half), ("xt", b)], [("xnew", b, half)])
                XN = [("xnew", b, 0), ("xnew", b, 1)]
                dma("sp", s_xnew[r0:r0 + 128, :], xnew[b][:], XN, [], 3)
                act(junk[:], xnew[b][:], AF.Square, XN, ["junk", "ssq"], accum_out=ssq[:])
                rsqrt(ssq[:], rstd[:], 1024.0, ["ssq"], ["rstd"], "x2")
                act(xn2[:], xnew[b][:], AF.Copy, XN + ["rstd"], ["xn2"], scale=rstd[:, 0:1])
                for k in range(8):
                    tr(T32[:, k, :], xn2[:, k * 128:(k + 1) * 128], identf[:], ["xn2", "identf"], ["T32"])
                tt("dve", tmpf[:], T32, bcast(gs2[:], 2, [128, 8, 128]), ALU.mult, ["T32", "gs2"], ["tmpf"])
                tt("dve", h2f[:], tmpf[:], bcast(sh2[:], 2, [128, 8, 128]), ALU.add, ["tmpf", "sh2"], ["h2f"])
                cp("pool", h2b[b][:], h2f[:], ["h2f"], [("h2b", b)])
                dma("sp", s_h2[:, :, r0:r0 + 128], h2b[b][:], [("h2b", b)], [], 3)
                for k in range(8):
                    mm(PB[:, 4, 0:NE], h2f[:, k, :], wr[:, k, :], k == 0, k == 7, ["h2f", "wr"], [("pb", 4)])
                tt("dve", lg[:], PB[:, 4, 0:NE], rows_t[:, R_BR:R_BR + NE], ALU.add, [("pb", 4), "rows"], ["lg"])
                P.op("dve", lambda e: e.max(out=mx8[:], in_=lg[:]), reads=["lg"], writes=["mx8"])
                ts("dve", msk[:], lg[:], mx8[:, 3:4], None, ALU.is_ge, None, ["lg", "mx8"], ["msk"])
                ts("dve", nmx[:], mx8[:, 0:1], -1.0, None, ALU.mult, None, ["mx8"], ["nmx"])
                act(ee[:], lg[:], AF.Exp, ["lg", "nmx"], ["ee"], bias=nmx[:, 0:1], scale=1.0)
                tt("dve", ee[:], ee[:], msk[:], ALU.mult, ["ee", "msk"], ["ee"])
                P.op("dve", lambda e: e.reduce_sum(out=esum[:], in_=ee[:], axis=AX.X), reads=["ee"], writes=["esum"])
                P.op("dve", lambda e: e.reciprocal(out=esum[:], in_=esum[:]), reads=["esum"], writes=["esum"])
                ts("dve", wts[:], ee[:], esum[:, 0:1], None, ALU.mult, None, ["ee", "esum"], ["wts"])
                tr(PB[0:32, 4, 256:384], wts[:], identf[:], ["wts", "identf"], [("pb", 4)])
                cp("act", WT[:, r0:r0 + 128], PB[0:32, 4, 256:384], [("pb", 4)], [("WT", r0)])
                cp("dve", WTb[:, r0:r0 + 128], PB[0:32, 4, 256:384], [("pb", 4)], [("WTb", r0)])
        if dbg:
            dma("sp", s_wt, WT[:], [("WT", r) for r in range(0, NTOK, 128)], [], 3)
        P.barrier()
        A.reset(markA)

    if stage >= 4:
        wgu = [A([128, 8, 2 * D], BF16) for _ in range(2)]
        wdn = A([128, 8, D], BF16)
        bdn = A([32, D], BF16)
        h2c = A([128, 8, 1024], BF16)
        yacc = A([128, 8, D], F32)
        actT = [A([128, 8, 512], BF16) for _ in range(2)]
        wbc = [A([128, 512], F32) for _ in range(2)]
        selw = [A([32, 128], F32) for _ in range(2)]
        gl = A([128, 512], F32)
        sgm = A([128, 512], F32)
        lb = A([128, 512], F32)
        tA = A([128, 512], F32)
        tB = A([128, 512], F32)
        xo = [A([128, D], F32) for _ in range(1)]
        fo = [A([128, D], F32) for _ in range(2)]
        dma("pool", bdn[:], bdn_d, [], ["bdn"], 1)
        units = [(tc, e, sub) for tc in range(4) for e in range(NE) for sub in range(2)]
        wgu_v = wgu_d.rearrange("e (k p) n -> e p k n", p=128)
        wdn_v = wdn_d.rearrange("e (k p) n -> e p k n", p=128)
        gctr = [0]

        def load_w(tc, e):
            b = e % 2
            for k in range(8):
                for c2 in range(2):
                    dma("pool", wgu[b][:, k, c2 * D:(c2 + 1) * D], wgu_v[e, :, k, c2 * D:(c2 + 1) * D], [], [("wgu", b, k)], 5 if b == 0 else 8)

        def load_dn(e):
            for k2 in range(4):
                dma("pool", wdn[:, 2 * k2:2 * k2 + 2, :], wdn_v[e, :, 2 * k2:2 * k2 + 2, :], [], [("wdn", k2)], 6)

        def GU(ui):
            tc, e, sub = units[ui]
            b = e % 2
            ab = ui % 2
            t0 = tc * 1024 + sub * 512
            hs = slice(sub * 512, (sub + 1) * 512)
            cp("dve", selw[ab][:], identf[0:32, e:e + 1].to_broadcast([32, 128]), ["identf"], [("selw", ab)])
            mm(PB[:, 6, :], selw[ab][:], WT[:, t0:t0 + 512], True, True,
               [("WT", r) for r in range(t0, t0 + 512, 128)] + [("selw", ab)], [("pb", 6)])
            cp("act", wbc[ab][:], PB[:, 6, :], [("pb", 6)], [("wbc", ab)])
            for j in range(8):
                gb = gctr[0] % 2
                gctr[0] += 1
                for k in range(8):
                    mm(PB[:, gb, :], wgu[b][:, k, j * 128:(j + 1) * 128], h2c[:, k, hs], k == 0, k == 7, [("wgu", b, k), "h2c"], [("pb", gb)])
                for k in range(8):
                    mm(PB[:, 2 + gb, :], wgu[b][:, k, D + j * 128:D + (j + 1) * 128], h2c[:, k, hs], k == 0, k == 7,
                       [("wgu", b, k), "h2c"], [("pb", 2 + gb)])
                cg = C_BGU + e * 16 + j
                ts("dve", gl[:], PB[:, gb, :], cols_t[:, cg:cg + 1], 7.0, ALU.add, ALU.min, [("pb", gb), "cols"], ["gl"])
                act(sgm[:], gl[:], AF.Sigmoid, ["gl"], ["sgm"], scale=1.702)
                act(lb[:], PB[:, 2 + gb, :], AF.Identity, [("pb", 2 + gb), "cols"], ["lb"], bias=cols_t[:, cg + 8:cg + 9], scale=1.0)
                ts("dve", lb[:], lb[:], -7.0, 7.0, ALU.max, ALU.min, ["lb"], ["lb"])
                tt("dve", tA[:], gl[:], sgm[:], ALU.mult, ["gl", "sgm"], ["tA"])
                stt("dve", tB[:], lb[:], 1.0, tA[:], ALU.add, ALU.mult, ["lb", "tA"], ["tB"])
                tt("dve", actT[ab][:, j, :], tB[:], wbc[ab][:], ALU.mult, ["tB", ("wbc", ab)], [("actT", ab, j)])

        def DN(ui):
            tc, e, sub = units[ui]
            ab = ui % 2
            AT = [("actT", ab, j) for j in range(8)]
            for t in range(4):
                tg = sub * 4 + t
                r0 = tc * 1024 + tg * 128
                for half in range(2):
                    hs = slice(half * 512, (half + 1) * 512)
                    bk = 4 + half
                    if e == 0:
                        mm(PB[:, bk, :], WTb[:, r0:r0 + 128], bdn[:, hs], True, False, [("WTb", r0), "bdn"], [("pb", bk)])
                    for j in range(8):
                        mm(PB[:, bk, :], actT[ab][:, j, t * 128:(t + 1) * 128], wdn[:, j, hs], (j == 0 and e != 0), j == 7,
                           AT + [("wdn", j // 2)], [("pb", bk)])
                    if e == 0:
                        cp("act", yacc[:, tg, hs], PB[:, bk, :], [("pb", bk)], [("yacc", tg, half)])
                    else:
                        tt("dve", yacc[:, tg, hs], yacc[:, tg, hs], PB[:, bk, :], ALU.add, [("pb", bk), ("yacc", tg, half)], [("yacc", tg, half)])

        def FIN(tc):
            for tg in range(8):
                b = tg % 2
                r0 = tc * 1024 + tg * 128
                dma("sp", xo[0][:], s_xnew[r0:r0 + 128, :], [], [("xo", 0)], 2)
                tt("dve", fo[b][:], yacc[:, tg, :], g2bc[:], ALU.mult, [("yacc", tg, 0), ("yacc", tg, 1), ("g2bc", 0), ("g2bc", 1)], [("fo", b)])
                tt("dve", fo[b][:], fo[b][:], xo[0][:], ALU.add, [("fo", b), ("xo", 0)], [("fo", b)])
                dma("sp", out_d[r0:r0 + 128, :], fo[b][:], [("fo", b)], [], 7)

        nU = len(units)
        for ui in range(nU + 1):
            if ui < nU:
                tc, e, sub = units[ui]
                if sub == 0:
                    if e == 0:
                        dma("sp", h2c[:], s_h2[:, :, tc * 1024:(tc + 1) * 1024], [], ["h2c"], 2)
                    if ui == 0:
                        load_w(tc, 0)
                    ne = ui + 2
                    if ne < nU:
                        load_w(units[ne][0], units[ne][1])
                    if ui == 0:
                        load_dn(0)
                GU(ui)
            if ui >= 1:
                DN(ui - 1)
                ptc, pe_, psub = units[ui - 1]
                if pe_ == NE - 1 and psub == 1:
                    FIN(ptc)
            if ui < nU and ui >= 1 and units[ui][2] == 0:
                load_dn(units[ui][1])
    P.emit()
    return nc, P


def _rope_tables(dim):
    q = dim // 4
    n = NTOK
    row = np.repeat(np.arange(n // 64, dtype=np.int32), 64).astype(np.float32)
    col = np.tile(np.arange(64, dtype=np.int32), n // 64).astype(np.float32)
    freqs = (np.float32(10000.0) ** (-np.arange(q, dtype=np.float32) / np.float32(q))).astype(np.float32)
    ar = row[:, None] * freqs
    ac = col[:, None] * freqs
    ang = np.concatenate([ar, ar, ac, ac], axis=-1).astype(np.float32)
    cos = np.cos(ang).astype(np.float32)
    sin = np.sin(ang).astype(np.float32)
    sgn = np.concatenate([-np.ones(q), np.ones(q), -np.ones(q), np.ones(q)]).astype(np.float32)
    return cos, sin * sgn


def _prep(inp):
    f = lambda a: np.ascontiguousarray(np.asarray(a, dtype=np.float32))
    L = 0
    cm, sm = _rope_tables(32)
    cs, ss = _rope_tables(64)
    rope = f(np.concatenate([cm, sm, cs, ss], axis=1).reshape(32, 128, 192))
    kj = np.arange(128)[:, None]
    qi = np.arange(128)[None, :]
    masks = f(np.concatenate([(qi <= kj), (qi >= kj)], axis=1))
    bgu = np.asarray(inp["b_gate_up"][L], np.float32)
    bgu_cols = np.zeros((128, 512), np.float32)
    for e in range(NE):
        bgu_cols[:, e * 16:e * 16 + 8] = bgu[e, 0::2].reshape(8, 128).T
        bgu_cols[:, e * 16 + 8:e * 16 + 16] = bgu[e, 1::2].reshape(8, 128).T
    wgu = np.asarray(inp["w_gate_up"][L], np.float32)
    wgu_p = f(np.concatenate([wgu[:, :, 0::2], wgu[:, :, 1::2]], axis=2))
    shared = dict(
        rope=rope, masks=masks,
        w_ada=f(inp["w_ada"][L]), w_in=f(inp["w_in"][L]), w_q_up=f(inp["w_q_up"][L]), w_kv_up=f(inp["w_kv_up"][L]),
        w_branch_a=f(inp["w_branch_a"][L]), w_branch_b=f(inp["w_branch_b"][L]), w_out=f(inp["w_out"][L]),
        w_router=f(inp["w_router"][L]), w_gu=wgu_p, w_dn=f(inp["w_down"][L]), b_dn=f(inp["b_down"][L]),
    )
    b_ada = np.asarray(inp["b_ada"][L], np.float32)
    rep = lambda v: np.broadcast_to(np.asarray(v, np.float32)[None, :], (128, len(v)))
    rows = f(np.concatenate([
        rep(inp["mla_q_g"][L]), rep(inp["mla_k_g"][L]), rep(inp["swa_q_g"][L]), rep(inp["swa_k_g"][L]),
        rep(inp["b_router"][L]), rep(np.repeat(np.asarray(inp["swa_sink"][L], np.float32), 128)),
        rep(b_ada[2048:3072]), rep(b_ada[5120:6144])], axis=1))
    assert rows.shape == (128, NROW)
    in_maps = []
    for b in range(8):
        colT = lambda v: np.asarray(v, np.float32).reshape(-1, 128).T
        cols = f(np.concatenate([
            colT(b_ada), colT(inp["norm1_g"][L]), colT(inp["norm2_g"][L]), colT(inp["c"][b]), colT(inp["c_ctx"]),
            colT(inp["mla_q_a_g"][L]), colT(inp["mla_kv_a_g"][L]), bgu_cols], axis=1))
        assert cols.shape == (128, NCOL)
        m = dict(shared)
        m.update(x=f(inp["x"][b]), ctx=f(inp["ctx"][b]), cols=cols, rows=rows)
        in_maps.append(m)
    return in_maps


_CACHE = {}


def kernel(**inputs):
    in_maps = _prep(inputs)
    if "nc" not in _CACHE:
        _CACHE["nc"] = build()[0]
    res = run_bass_kernel_spmd(_CACHE["nc"], in_maps, core_ids=list(range(8)))
    return np.stack([np.asarray(r["out"], dtype=np.float32) for r in res.results], axis=0)
```

```python
import contextlib
import numpy as np
import concourse.bass as bass
import concourse.mybir as mybir
from concourse.bass_utils import run_bass_kernel_spmd

F32 = mybir.dt.float32
BF16 = mybir.dt.bfloat16
AF = mybir.ActivationFunctionType
ALU = mybir.AluOpType
AX = mybir.AxisListType

ENGS = ("pe", "act", "dve", "pool", "sp")
D = 1024
NTOK = 4096
NCTX = 256
NKEY = NTOK + NCTX
NKT = NKEY // 128
NE = 32
EPS = 1e-6

C_BADA, C_G1, C_G2, C_C, C_QAG, C_KVAG, C_BGU = 0, 48, 56, 64, 80, 83, 85
NCOL = 85 + 512
R_QG, R_KG, R_SQG, R_SKG, R_BR, R_SINK, R_G1B, R_G2B = 0, 96, 192, 256, 320, 352, 1376, 2400
NROW = 3424


class Prog:
    def __init__(self, nc, same_engine_sync=True):
        self.nc = nc
        self.ops = []
        self.last_w = {}
        self.readers = {}
        self.same_engine_sync = same_engine_sync

    mute = False
    RING = 8

    def op(self, eng, fn, reads=(), writes=(), stream=None):
        if self.mute:
            return None
        _ps = lambda r: r in ("pmod", "T0", "pm0", "pm1", "pkv", "T32") or (isinstance(r, tuple) and r[0] == "pb")
        writes = list(writes) + [r for r in reads if _ps(r)]
        reads = [r for r in reads if not _ps(r)]
        idx = len(self.ops)
        deps = set()
        for r in reads:
            if r in self.last_w:
                deps.add(self.last_w[r])
        for w in writes:
            if w in self.last_w:
                deps.add(self.last_w[w])
            for rd in self.readers.get(w, ()):
                deps.add(rd)
        for w in writes:
            self.last_w[w] = idx
            self.readers[w] = []
        for r in reads:
            self.readers.setdefault(r, []).append(idx)
        deps.discard(idx)
        self.ops.append(dict(eng=eng, fn=fn, deps=deps, stream=stream, inc=False))
        return idx

    def barrier(self):
        self.mute = False
        last = {}
        cnts = {}
        for i, o in enumerate(self.ops):
            if o["fn"] is None:
                continue
            if o["stream"] is not None:
                c = cnts.get(o["stream"], 0)
                cnts[o["stream"]] = c + 1
                key = ("s", o["stream"], c % self.RING)
            else:
                key = ("e", o["eng"])
            last[key] = i
        deps = set(last.values())
        for e in ENGS:
            self.ops.append(dict(eng=e, fn=None, deps=set(deps), stream=None, inc=False))
        self.last_w = {}
        self.readers = {}

    def _skip(self, od, o):
        return (od["stream"] is None and o["stream"] is None and od["eng"] == o["eng"]
                and (od["eng"] == "pe" or not self.same_engine_sync))

    def emit(self):
        nc = self.nc
        ops = self.ops
        for o in ops:
            for d in o["deps"]:
                od = ops[d]
                if od["stream"] is None and not self._skip(od, o):
                    od["inc"] = True
        cnt = {}
        scnt = {}
        ring_last = {}
        for i, o in enumerate(ops):
            if o["stream"] is not None:
                c = scnt.get(o["stream"], 0)
                scnt[o["stream"]] = c + 1
                k = ("s", o["stream"], c % self.RING)
                cnt[k] = cnt.get(k, 0) + 16
                o["done"] = (k, cnt[k])
                if k in ring_last:
                    o["deps"].add(ring_last[k])
                ring_last[k] = i
            elif o["inc"]:
                k = ("e", o["eng"])
                cnt[k] = cnt.get(k, 0) + 1
                o["done"] = (k, cnt[k])
            else:
                o["done"] = None
        self.final_counts = dict(cnt)
        with contextlib.ExitStack() as st:
            sems = {}
            for k in cnt:
                sems[k] = st.enter_context(nc.semaphore("sem_" + "_".join(str(v) for v in k)))
            block = st.enter_context(nc.Block())

            def run_engine(ename, handle):
                known = {}
                for o in ops:
                    if o["eng"] != ename:
                        continue
                    need = {}
                    for d in o["deps"]:
                        od = ops[d]
                        if od["done"] is None or self._skip(od, o):
                            continue
                        k, v = od["done"]
                        if need.get(k, 0) < v:
                            need[k] = v
                    for k, v in need.items():
                        if known.get(k, 0) < v:
                            handle.wait_ge(sems[k], v)
                            known[k] = v
                    if o["fn"] is None:
                        continue
                    ins = o["fn"](handle)
                    if o["done"] is not None:
                        k, v = o["done"]
                        ins.then_inc(sems[k], 16 if k[0] == "s" else 1)
                if ename == "sp":
                    for k, v in cnt.items():
                        if k[0] == "s" and known.get(k, 0) < v:
                            handle.wait_ge(sems[k], v)

            @block.tensor
            def _(e):
                run_engine("pe", e)

            @block.scalar
            def _(e):
                run_engine("act", e)

            @block.vector
            def _(e):
                run_engine("dve", e)

            @block.gpsimd
            def _(e):
                run_engine("pool", e)

            @block.sync
            def _(e):
                run_engine("sp", e)


class Alloc:
    def __init__(self, nc):
        self.nc = nc
        self.top = (int(nc._sbuf_addr_for_side("left")) + 63) // 64 * 64
        self.limit = int(nc._sbuf_addr_for_side("right")) // 64 * 64
        self.n = 0

    def __call__(self, shape, dt, name=None):
        per = int(np.prod(shape[1:])) * (2 if dt == BF16 else 4)
        per = (per + 63) // 64 * 64
        off = self.top
        self.top += per
        assert self.top <= self.limit, f"SBUF overflow {self.top}"
        self.n += 1
        return self.nc.alloc_sbuf_tensor_at(name or f"t{self.n}_{off}", list(shape), dt, offset=off)

    def mark(self):
        return self.top

    def reset(self, m):
        self.top = m


def bcast(ap, axis, shape):
    return ap.unsqueeze(axis).to_broadcast(list(shape))


def build(stage=99, dbg=False):
    nc = bass.Bass("TRN2", target_bir_lowering=False)
    P = Prog(nc)

    def din(name, shape, dt=F32):
        return nc.dram_tensor(name, list(shape), dt, kind="ExternalInput").ap()

    def dscr(name, shape, dt):
        return nc.dram_tensor(name, list(shape), dt, kind=("ExternalOutput" if dbg else "Internal")).ap()

    x_d = din("x", [NTOK, D])
    ctx_d = din("ctx", [NCTX, D])
    cols_d = din("cols", [128, NCOL])
    rows_d = din("rows", [128, NROW])
    rope_d = din("rope", [32, 128, 192])
    mask_d = din("masks", [128, 256])
    wada_d = din("w_ada", [D, 6 * D])
    win_d = din("w_in", [D, 3488])
    wqup_d = din("w_q_up", [384, 768])
    wkvup_d = din("w_kv_up", [256, 1024])
    wba_d = din("w_branch_a", [512, D])
    wbb_d = din("w_branch_b", [512, D])
    wout_d = din("w_out", [D, D])
    wr_d = din("w_router", [D, NE])
    wgu_d = din("w_gu", [NE, D, 2 * D])
    wdn_d = din("w_dn", [NE, D, D])
    bdn_d = din("b_dn", [NE, D])
    out_d = nc.dram_tensor("out", [NTOK, D], F32, kind="ExternalOutput").ap()

    s_kt = dscr("s_kt", [96, 8, NKEY], BF16)
    s_va = dscr("s_va", [8, 128, NKT, 65], BF16)
    s_kst = dscr("s_kst", [128, NKEY], BF16)
    s_vsa = dscr("s_vsa", [128, NKT, 130], BF16)
    s_qm = dscr("s_qm", [96, 8, NTOK], BF16)
    s_qs = dscr("s_qs", [128, 32, 512], BF16)
    s_gate = dscr("s_gate", [128, 16, NTOK], BF16)
    s_o = dscr("s_o", [64, 16, NTOK], BF16)
    s_xnew = dscr("s_xnew", [NTOK, D], F32)
    s_h2 = dscr("s_h2", [128, 8, NTOK], BF16)
    if dbg:
        s_wt = dscr("s_wt", [32, NTOK], F32)

    A = Alloc(nc)
    T0t = nc.alloc_psum_tensor("t0", [128, 8, 128], BF16)
    T0b = T0t[:, :, :]
    PB = nc.alloc_psum_tensor("pb", [128, 7, 512], F32)

    def bank2(i):
        return PB[:, i:i + 2, :].rearrange("p a b -> p (a b)")

    def dma(eng, out, in_, reads, writes, stream):
        P.op(eng, lambda e: e.dma_start(out=out, in_=in_), reads=reads, writes=writes, stream=stream)

    def mm(out, lhsT, rhs, start, stop, reads, writes):
        P.op("pe", lambda e: e.matmul(out, lhsT=lhsT, rhs=rhs, start=start, stop=stop), reads=reads, writes=writes)

    def tr(out, in_, ident, reads, writes):
        P.op("pe", lambda e: e.transpose(out=out, in_=in_, identity=ident), reads=reads, writes=writes)

    def act(out, in_, func, reads, writes, **kw):
        P.op("act", lambda e: e.activation(out=out, in_=in_, func=func, **kw), reads=reads, writes=writes)

    def tt(eng, out, in0, in1, op, reads, writes):
        P.op(eng, lambda e: e.tensor_tensor(out=out, in0=in0, in1=in1, op=op), reads=reads, writes=writes)

    def ts(eng, out, in0, s1, s2, op0, op1, reads, writes):
        if s2 is None:
            P.op(eng, lambda e: e.tensor_scalar(out=out, in0=in0, scalar1=s1, scalar2=None, op0=op0), reads=reads, writes=writes)
        else:
            P.op(eng, lambda e: e.tensor_scalar(out=out, in0=in0, scalar1=s1, scalar2=s2, op0=op0, op1=op1), reads=reads, writes=writes)

    def stt(eng, out, in0, scalar, in1, op0, op1, reads, writes):
        P.op(eng, lambda e: e.scalar_tensor_tensor(out=out, in0=in0, scalar=scalar, in1=in1, op0=op0, op1=op1),
             reads=reads, writes=writes)

    def cp(eng, out, in_, reads, writes):
        if eng == "act":
            P.op("act", lambda e: e.copy(out=out, in_=in_), reads=reads, writes=writes)
        else:
            P.op(eng, lambda e: e.tensor_copy(out=out, in_=in_), reads=reads, writes=writes)

    def rsqrt(src, dst, n, reads, writes, tag):
        act(dst, src, AF.Sqrt, reads, writes, scale=1.0 / n, bias=EPS)
        P.op("dve", lambda e: e.reciprocal(out=dst, in_=dst), reads=writes, writes=writes)

    cols_t = A([128, NCOL], F32, "cols_t")
    g2bc = A([128, D], F32, "g2bc")
    WT = A([32, NTOK], F32, "WT")
    WTb = A([32, NTOK], BF16, "WTb")
    Wtok = A([128, 32, NE], F32, "Wtok")
    identf = A([128, 128], F32, "identf")
    ident = A([128, 128], BF16, "ident")
    eps_t = A([128, 1], F32, "eps_t")
    gs2 = A([128, 8], F32, "gs2")
    sh2 = A([128, 8], F32, "sh2")
    markA = A.mark()
    rows_t = A([128, NROW], F32, "rows_t")
    g1bc = A([128, D], F32, "g1bc")
    modT = A([128, 48, 2], F32, "modT")
    gs1 = A([128, 8, 2], F32, "gs1")
    onesb = A([128, 128], BF16, "onesb")
    onesf = A([128, 64], F32, "onesf")
    maskb = A([128, 256], BF16, "maskb")
    markB = A.mark()

    dma("sp", cols_t[:], cols_d, [], ["cols"], 0)
    dma("sp", rows_t[:], rows_d, [], ["rows"], 0)
    maskf = A([128, 256], F32)
    dma("sp", maskf[:], mask_d, [], ["maskf"], 0)
    P.op("pool", lambda e: e.memset(identf[:], 0.0), writes=["identf"])
    P.op("pool", lambda e: e.affine_select(out=identf[:], in_=identf[:], pattern=[[-1, 128]], compare_op=ALU.not_equal,
                                            fill=1.0, base=0, channel_multiplier=1), reads=["identf"], writes=["identf"])
    cp("dve", ident[:], identf[:], ["identf"], ["ident"])
    cp("dve", maskb[:], maskf[:], ["maskf"], ["maskb"])
    P.op("pool", lambda e: e.memset(onesb[:], 1.0), writes=["onesb"])
    P.op("pool", lambda e: e.memset(onesf[:], 1.0), writes=["onesf"])
    P.op("pool", lambda e: e.memset(eps_t[:], EPS), writes=["eps"])
    sc = A([128, 8, 2], F32)
    scB = A([128, 8, 128], F32)
    act(sc[:, :, 0], cols_t[:, C_C:C_C + 8], AF.Silu, ["cols"], ["sc0"])
    act(sc[:, :, 1], cols_t[:, C_C + 8:C_C + 16], AF.Silu, ["cols"], ["sc1"])
    cp("dve", scB[:], sc[:, :, 0:1].to_broadcast([128, 8, 128]), ["sc0"], ["scB"])
    wa = [A([128, 8, 512], F32) for _ in range(2)]
    wada_v = wada_d.rearrange("(k p) n -> p k n", p=128)
    pmod = PB[:, 0, 0:96].rearrange("p (j s) -> p j s", s=2)
    bcbank = {4: 1, 5: 2, 10: 3, 11: 4}
    for cc in range(12):
        b = cc % 2
        dma("sp", wa[b][:], wada_v[:, :, cc * 512:(cc + 1) * 512], [], [("wa", b)], 0)
        for jj in range(4):
            j = cc * 4 + jj
            for k in range(8):
                mm(pmod[:, j, :], wa[b][:, k, jj * 128:(jj + 1) * 128], sc[:, k, :], k == 0, k == 7,
                   [("wa", b), "sc0", "sc1"], ["pmod"])
        if cc in bcbank:
            bk = bcbank[cc]
            for k in range(8):
                mm(PB[:, bk, :], scB[:, k, :], wa[b][:, k, :], k == 0, k == 7, [("wa", b), "scB"], [("pb", bk)])
            half = cc % 2
            if cc < 6:
                tt("dve", g1bc[:, half * 512:(half + 1) * 512], PB[:, bk, :], rows_t[:, R_G1B + half * 512:R_G1B + (half + 1) * 512],
                   ALU.add, [("pb", bk), "rows"], [("g1bc", half)])
            else:
                tt("dve", g2bc[:, half * 512:(half + 1) * 512], PB[:, bk, :], rows_t[:, R_G2B + half * 512:R_G2B + (half + 1) * 512],
                   ALU.add, [("pb", bk), "rows"], [("g2bc", half)])
    tt("dve", modT[:], pmod, bcast(cols_t[:, C_BADA:C_BADA + 48], 2, [128, 48, 2]), ALU.add, ["pmod", "cols"], ["modT"])
    stt("dve", gs1[:], modT[:, 8:16, :], 1.0, bcast(cols_t[:, C_G1:C_G1 + 8], 2, [128, 8, 2]), ALU.add, ALU.mult,
        ["modT", "cols"], ["gs1"])
    stt("dve", gs2[:], modT[:, 32:40, 0], 1.0, cols_t[:, C_G2:C_G2 + 8], ALU.add, ALU.mult, ["modT", "cols"], ["gs2"])
    cp("dve", sh2[:], modT[:, 24:32, 0], ["modT"], ["sh2"])
    P.barrier()
    A.reset(markB)

    if stage >= 1:
        win = A([128, 8, 3488], BF16, "win")
        wkv = A([128, 2, 1024], BF16, "wkv")
        wq = A([128, 3, 768], BF16, "wq")
        win_v = win_d.rearrange("(k p) n -> p k n", p=128)
        import os as _os
        SKIP = _os.environ.get('K_SKIP', '')
        if 'w' in SKIP:
            P.mute = True
        for k in range(8):
            for c4 in range(4):
                dma("pool", win[:, k, c4 * 872:(c4 + 1) * 872], win_v[:, k, c4 * 872:(c4 + 1) * 872], [], [("win", k)], 1)
        dma("pool", wkv[:], wkvup_d.rearrange("(k p) n -> p k n", p=128), [], ["wkv"], 1)
        dma("pool", wq[:], wqup_d.rearrange("(k p) n -> p k n", p=128), [], ["wq"], 1)
        WIN = [("win", k) for k in range(8)]
        P.mute = False
        xt = [A([128, D], F32) for _ in range(2)]
        junk = A([128, D], F32)
        ssq = A([128, 1], F32)
        rstd = A([128, 1], F32)
        xn = A([128, D], BF16)
        tmpf = A([128, 8, 128], F32)
        hT = [A([128, 8, 512], BF16) for _ in range(1)]
        latf = A([128, 5, 512], F32)
        sq = A([128, 5, 512], BF16)
        rbc = A([128, 512], F32)
        latn = A([128, 5, 512], BF16)
        sg = A([128, 8, 512], BF16)
        ropet = [A([128, 192], F32) for _ in range(2)]
        scr = A([128, 768], F32)
        scr2 = A([128, 768], F32)
        rtA = A([128, 512], F32)
        rtB = A([128, 512], F32)
        st8 = A([128, 8], F32)
        ssqr = A([128, 1], F32)
        kn = A([128, 8, 96], BF16)
        krg = A([128, 32], F32)
        krr = A([128, 32], F32)
        ktile = A([128, 8, 128], BF16)
        vtile = [A([128, 8, 65], BF16) for _ in range(2)]
        qb = A([128, 8, 96], BF16)
        qtile = A([128, 8, 128], BF16)
        ksb = A([128, 128], BF16)
        kstile = A([128, 128], BF16)
        vstile = [A([128, 2, 65], BF16) for _ in range(2)]
        qsb = A([128, 8, 64], BF16)
        qsbP = A([128, 4, 2, 64], BF16)
        qstile = A([128, 4, 128], BF16)
        for b in range(0 if 'm' not in SKIP else 2, 2):
            P.op("pool", lambda e, b=b: e.memset(vtile[b][:], 1.0), writes=[("vtile", b)])
            P.op("pool", lambda e, b=b: e.memset(vstile[b][:], 1.0), writes=[("vstile", b)])

        def rope(src3, H, R, cos, ss, dst3, rd, wr):
            q = R // 4
            a3 = rtA[:, 0:H * R].rearrange("p (h r) -> p h r", h=H)
            b3 = rtB[:, 0:H * R].rearrange("p (h r) -> p h r", h=H)
            tt("pool", a3, src3, bcast(cos, 1, [128, H, R]), ALU.mult, rd, ["rtA"])
            s5 = src3.rearrange("p h (a b c) -> p h a b c", a=2, b=2)
            b5 = b3.rearrange("p h (a b c) -> p h a b c", a=2, b=2)
            ss4 = ss.rearrange("p (a b c) -> p a b c", a=2, b=2)
            tt("pool", b5[:, :, :, 0, :], s5[:, :, :, 1, :], bcast(ss4[:, :, 0, :], 1, [128, H, 2, q]), ALU.mult, rd, ["rtB0"])
            tt("pool", b5[:, :, :, 1, :], s5[:, :, :, 0, :], bcast(ss4[:, :, 1, :], 1, [128, H, 2, q]), ALU.mult, rd, ["rtB1"])
            tt("pool", dst3, a3, b3, ALU.add, ["rtA", "rtB0", "rtB1"], wr)

        def headnorm(src3, H, Dh, gain, dstf3, rd, tag, extra=None):
            s3 = scr[:, 0:H * Dh].rearrange("p (h d) -> p h d", h=H)
            act(s3, src3, AF.Square, rd, ["scr"])
            P.op("dve", lambda e: e.reduce_sum(out=st8[:, 0:H], in_=s3, axis=AX.X), reads=["scr"], writes=["st8"])
            if extra is not None:
                ts("dve", st8[:, 0:H], st8[:, 0:H], extra[0], None, ALU.add, None, ["st8", extra[1]], ["st8"])
            rsqrt(st8[:, 0:H], st8[:, 0:H], float(Dh if extra is None else 96), ["st8"], ["st8"], tag)
            tt("dve", s3, src3, bcast(st8[:, 0:H], 2, [128, H, Dh]), ALU.mult, rd + ["st8", "scr"], ["scr"])
            tt("pool", dstf3, s3, bcast(gain, 1, [128, H, Dh]), ALU.mult, ["scr", "rows"], [tag])

        supers = [(ctx_d, 0, 2, 1, False, 0)] + [(x_d, i * 512, 4, 0, True, 2 + 4 * i) for i in range(8)]
        import os as _os
        supers = supers[:int(_os.environ.get('K_NSUP', '9'))]
        CUT = int(_os.environ.get('K_CUT', '99'))
        tile_ctr = 0
        for si, (src, row0, NT, sidx, latent, kt0) in enumerate(supers):
            NTK = NT * 128
            hb = 0
            for t in range(NT):
                b = tile_ctr % 2
                tile_ctr += 1
                dma("sp", xt[b][:], src[row0 + t * 128:row0 + (t + 1) * 128, :], [], [("xt", b)], 2)
                act(junk[:], xt[b][:], AF.Square, [("xt", b)], ["junk", "ssq"], accum_out=ssq[:])
                rsqrt(ssq[:], rstd[:], 1024.0, ["ssq"], ["rstd"], "x")
                act(xn[:], xt[b][:], AF.Copy, [("xt", b), "rstd"], ["xn"], scale=rstd[:, 0:1])
                for k in range(8):
                    tr(T0b[:, k, :], xn[:, k * 128:(k + 1) * 128], ident[:], ["xn", "ident"], ["T0"])
                tt("dve", tmpf[:], T0b, bcast(gs1[:, :, sidx], 2, [128, 8, 128]), ALU.mult, ["T0", "gs1"], ["tmpf"])
                tt("dve", hT[hb][:, :, t * 128:(t + 1) * 128], tmpf[:], bcast(modT[:, 0:8, sidx], 2, [128, 8, 128]), ALU.add,
                   ["tmpf", "modT"], [("hT", hb, t)])
            HT = [("hT", hb, t) for t in range(NT)]
            if CUT == 1:
                P.mute = True
            chunks = [(0, 0), (1, 128)] + ([(2, 544), (3, 672), (4, 800)] if latent else [])
            fb = 0
            for ci, c0 in chunks:
                bk = 1 + (fb % 2)
                fb += 1
                for k in range(8):
                    mm(PB[:, bk, 0:NTK], win[:, k, c0:c0 + 128], hT[hb][:, k, 0:NTK], k == 0, k == 7, HT + [("win", k)], [("pb", bk)])
                cp("dve", latf[:, ci, 0:NTK], PB[:, bk, 0:NTK], [("pb", bk)], [("latf", ci)])
                act(sq[:, ci, 0:NTK], PB[:, bk, 0:NTK], AF.Square, [("pb", bk)], [("sq", ci)])
            for (cis, n, goff) in ([([0, 1], 256.0, C_KVAG)] + ([([2, 3, 4], 384.0, C_QAG - 2)] if latent else [])):
                bk = 1 + (fb % 2)
                fb += 1
                for i, ci in enumerate(cis):
                    mm(PB[:, bk, 0:NTK], onesb[:], sq[:, ci, 0:NTK], i == 0, i == len(cis) - 1, [("sq", ci), "onesb"], [("pb", bk)])
                rsqrt(PB[:, bk, 0:NTK], rbc[:, 0:NTK], n, [("pb", bk)], ["rbc"], "lat")
                for ci in cis:
                    stt("dve", latn[:, ci, 0:NTK], latf[:, ci, 0:NTK], cols_t[:, goff + ci:goff + ci + 1], rbc[:, 0:NTK],
                        ALU.mult, ALU.mult, [("latf", ci), "rbc", "cols"], [("latn", ci)])
            if latent:
                for gc in range(16):
                    bk = 1 + (fb % 2)
                    fb += 1
                    c0 = 1440 + gc * 128
                    for k in range(8):
                        mm(PB[:, bk, :], win[:, k, c0:c0 + 128], hT[hb][:, k, :], k == 0, k == 7, HT + [("win", k)], [("pb", bk)])
                    act(sg[:, gc % 8, :], PB[:, bk, :], AF.Sigmoid, [("pb", bk)], [("sg", gc % 8)])
                    if gc % 8 == 7:
                        g0 = gc - 7
                        dma("sp", s_gate[:, g0:g0 + 8, row0:row0 + 512], sg[:], [("sg", i) for i in range(8)], [("sgall")], 3)
            if CUT == 2:
                P.mute = True
            pm = bank2(3)
            pkv = bank2(5)
            for t in range(NT):
                tsl = slice(t * 128, (t + 1) * 128)
                kt = kt0 + t
                key0 = kt * 128
                tok0 = row0 + t * 128
                vb = kt % 2
                if latent:
                    rb = kt % 2
                    dma("sp", ropet[rb][:], rope_d[kt - 2], [], [("rope", rb)], 2)
                    cos_m, ss_m = ropet[rb][:, 0:32], ropet[rb][:, 32:64]
                    cos_s, ss_s = ropet[rb][:, 64:128], ropet[rb][:, 128:192]
                    RP = [("rope", rb)]
                for k in range(8):
                    mm(pm[:, 0:288], hT[hb][:, k, tsl], win[:, k, 256:544], k == 0, k == 7, [("hT", hb, t), ("win", k)], ["pm0"])
                if latent:
                    for k in range(8):
                        mm(pm[:, 512:1024], hT[hb][:, k, tsl], win[:, k, 928:1440], k == 0, k == 7, [("hT", hb, t), ("win", k)], ["pm1"])
                for half in range(2):
                    for c in range(2):
                        mm(pkv[:, half * 512:(half + 1) * 512], latn[:, c, tsl], wkv[:, c, half * 512:(half + 1) * 512], c == 0, c == 1,
                           [("latn", c), "wkv"], ["pkv"])
                pkv3 = pkv.rearrange("p (h d) -> p h d", h=8)
                if CUT == 3:
                    P.mute = True
                act(junk[:, 0:32], pm[:, 0:32], AF.Square, ["pm0"], ["junk", "ssqr"], accum_out=ssqr[:])
                s3 = scr[:, 0:512].rearrange("p (h d) -> p h d", h=8)
                act(s3, pkv3[:, :, 0:64], AF.Square, ["pkv"], ["scr"])
                P.op("dve", lambda e, s3=s3: e.reduce_sum(out=st8[:], in_=s3, axis=AX.X), reads=["scr"], writes=["st8"])
                ts("dve", st8[:], st8[:], ssqr[:, 0:1], None, ALU.add, None, ["st8", "ssqr"], ["st8"])
                rsqrt(st8[:], st8[:], 96.0, ["st8"], ["st8"], "k")
                tt("dve", s3, pkv3[:, :, 0:64], bcast(st8[:], 2, [128, 8, 64]), ALU.mult, ["pkv", "st8", "scr"], ["scr"])
                tt("pool", kn[:, :, 0:64], s3, bcast(rows_t[:, R_KG:R_KG + 64], 1, [128, 8, 64]), ALU.mult, ["scr", "rows"], ["kn0"])
                tt("dve", krg[:], pm[:, 0:32], rows_t[:, R_KG + 64:R_KG + 96], ALU.mult, ["pm0", "rows"], ["krg"])
                if latent:
                    rope(krg[:].unsqueeze(1), 1, 32, cos_m, ss_m, krr[:].unsqueeze(1), ["krg"] + RP, ["krr"])
                    krsrc, KR = krr, "krr"
                else:
                    krsrc, KR = krg, "krg"
                tt("dve", kn[:, :, 64:96], bcast(krsrc[:], 1, [128, 8, 32]), bcast(st8[:], 2, [128, 8, 32]), ALU.mult, [KR, "st8"], ["kn1"])
                for h in range(8):
                    tr(T0b[0:96, h, :], kn[:, h, :], ident[:], ["kn0", "kn1", "ident"], ["T0"])
                cp("act", ktile[0:96, :, :], T0b[0:96, :, :], ["T0"], ["ktile"])
                dma("sp", s_kt[:, :, key0:key0 + 128], ktile[0:96, :, :], ["ktile"], [], 3)
                cp("act", vtile[vb][:, :, 0:64], pkv3[:, :, 64:128], ["pkv"], [("vtile", vb)])
                dma("sp", s_va.rearrange("h p t e -> p h t e")[:, :, kt, :], vtile[vb][:], [("vtile", vb)], [], 3)
                if CUT == 4:
                    P.mute = True
                pks = pm[:, 32:160].rearrange("p (h d) -> p h d", h=2)
                ksn = scr2[:, 0:128].rearrange("p (h d) -> p h d", h=2)
                headnorm(pks, 2, 64, rows_t[:, R_SKG:R_SKG + 64], ksn, ["pm0"], "scr2")
                ksb3 = ksb[:].rearrange("p (h d) -> p h d", h=2)
                if latent:
                    rope(ksn, 2, 64, cos_s, ss_s, ksb3, ["scr2"] + RP, ["ksb"])
                else:
                    cp("pool", ksb3, ksn, ["scr2"], ["ksb"])
                tr(T0b[:, 0, :], ksb[:], ident[:], ["ksb", "ident"], ["T0"])
                cp("act", kstile[:], T0b[:, 0, :], ["T0"], ["kstile"])
                dma("sp", s_kst[:, key0:key0 + 128], kstile[:], ["kstile"], [], 3)
                cp("act", vstile[vb][:, :, 0:64], pm[:, 160:288].rearrange("p (h d) -> p h d", h=2), ["pm0"], [("vstile", vb)])
                dma("sp", s_vsa[:, kt, :], vstile[vb][:].rearrange("p h e -> p (h e)"), [("vstile", vb)], [], 3)
                if not latent:
                    continue
                pq = pkv
                for (c0, c1) in ((0, 512), (512, 768)):
                    for c in range(3):
                        mm(pq[:, c0:c1], latn[:, 2 + c, tsl], wq[:, c, c0:c1], c == 0, c == 2, [("latn", 2 + c), "wq"], ["pkv"])
                pq3 = pq[:, 0:768].rearrange("p (h d) -> p h d", h=8)
                qn3 = scr2[:, 0:768].rearrange("p (h d) -> p h d", h=8)
                headnorm(pq3, 8, 96, rows_t[:, R_QG:R_QG + 96], qn3, ["pkv"], "scr2")
                cp("pool", qb[:, :, 0:64], qn3[:, :, 0:64], ["scr2"], ["qb0"])
                rope(qn3[:, :, 64:96], 8, 32, cos_m, ss_m, qb[:, :, 64:96], ["scr2"] + RP, ["qb1"])
                for h in range(8):
                    tr(T0b[0:96, h, :], qb[:, h, :], ident[:], ["qb0", "qb1", "ident"], ["T0"])
                cp("act", qtile[0:96, :, :], T0b[0:96, :, :], ["T0"], ["qtile"])
                dma("sp", s_qm[:, :, tok0:tok0 + 128], qtile[0:96, :, :], ["qtile"], [], 3)
                pqs = pm[:, 512:1024].rearrange("p (h d) -> p h d", h=8)
                qsn = scr2[:, 0:512].rearrange("p (h d) -> p h d", h=8)
                headnorm(pqs, 8, 64, rows_t[:, R_SQG:R_SQG + 64], qsn, ["pm1"], "scr2")
                rope(qsn, 8, 64, cos_s, ss_s, qsb[:], ["scr2"] + RP, ["qsb"])
                cp("pool", qsbP[:].rearrange("p hg g d -> p g hg d"), qsb[:].rearrange("p (g hg) d -> p g hg d", g=2), ["qsb"], ["qsbP"])
                for hg in range(4):
                    tr(T0b[:, hg, :], qsbP[:, hg, :, :].rearrange("p g d -> p (g d)"), ident[:], ["qsbP", "ident"], ["T0"])
                cp("act", qstile[:], T0b[:, 0:4, :], ["T0"], ["qstile"])
                dma("sp", s_qs[:, kt - 2, :], qstile[:].rearrange("p h q -> p (h q)"), ["qstile"], [], 3)
        P.barrier()
        A.reset(markB)

    if stage >= 2:
        kst = A([128, NKEY], BF16, "kst")
        vsa = A([128, NKT, 130], BF16, "vsa")
        esk = A([128, 1024], F32, "esk")
        dma("sp", kst[:], s_kst, [], ["kst"], 2)
        dma("sp", vsa[:], s_vsa, [], ["vsa"], 2)
        act(esk[:], rows_t[:, R_SINK:R_SINK + 1024], AF.Exp, ["rows"], ["esk"])
        kth = [A([128, NKEY], BF16) for _ in range(2)]
        vah = [A([128, NKT, 65], BF16) for _ in range(2)]
        qm = A([128, 8, 512], BF16)
        qs = A([128, 4, 512], BF16)
        NS = 5
        PT = [A([128, 512], BF16) for _ in range(NS)]
        oT = A([64, 16, 512], BF16)
        osb = A([64, 512], F32)
        rden = A([128, 512], F32)
        sctr = [0]
        octr = [0]
        hctr = 0

        def attend(tiles, qk_fn, pv_fn, masks, tag):
            n = len(tiles)
            ob = 5
            slots = []

            def issue_qk(i):
                sbk = sctr[0] % NS
                sctr[0] += 1
                qk_fn(tiles[i], PB[:, sbk, :], ("pb", sbk))
                slots.append(sbk)

            for i0 in range(min(n, NS - 1)):
                issue_qk(i0)
            for i in range(n):
                sbk = slots[i]
                act(PT[sbk][:], PB[:, sbk, :], AF.Exp, [("pb", sbk)], [("PT", sbk)], scale=tag)
                if masks[i] is not None:
                    m0 = masks[i]
                    p3 = PT[sbk][:].rearrange("p (h q) -> p h q", h=4)
                    tt("pool", p3, p3, bcast(maskb[:, m0:m0 + 128], 1, [128, 4, 128]), ALU.mult, [("PT", sbk), "maskb"], [("PT", sbk)])
                pv_fn(tiles[i], PB[0:65, ob, :], PT[sbk][:], i == 0, i == n - 1, ("PT", sbk), ("pb", ob))
                if i + NS - 1 < n:
                    issue_qk(i + NS - 1)
            return ob

        def finish(ob, dst, add_sink=None):
            if add_sink is not None:
                tt("dve", rden[64:65, :], PB[64:65, ob, :], add_sink, ALU.add, [("pb", ob), "esk"], ["rden"])
                P.op("dve", lambda e: e.reciprocal(out=rden[64:65, :], in_=rden[64:65, :]), reads=["rden"], writes=["rden"])
            else:
                P.op("dve", lambda e: e.reciprocal(out=rden[64:65, :], in_=PB[64:65, ob, :]), reads=[("pb", ob)], writes=["rden"])
            mm(PB[0:64, 6, :], onesf[64:65, 0:64], rden[64:65, :], True, True, ["rden", "onesf"], [("pb", 6)])
            cp("act", osb[:], PB[0:64, ob, :], [("pb", ob)], ["osb"])
            tt("dve", dst, osb[:] if len(dst.shape) == 2 else osb[:].rearrange("p (h q) -> p h q", h=4),
               PB[0:64, 6, :] if len(dst.shape) == 2 else PB[0:64, 6, :].rearrange("p (h q) -> p h q", h=4),
               ALU.mult, ["osb", ("pb", 6)], ["oT"])

        for ch in range(8):
            tok0 = ch * 512
            dma("sp", qm[0:96, :, :], s_qm[:, :, tok0:tok0 + 512], [], ["qm"], 2)
            dma("sp", qs[:], s_qs[:, ch * 4:(ch + 1) * 4, :], [], ["qs"], 2)
            for h in range(8):
                kb = hctr % 2
                hctr += 1
                dma("sp", kth[kb][0:96, :], s_kt[:, h, :], [], [("kth", kb)], 2)
                dma("sp", vah[kb][:], s_va[h], [], [("vah", kb)], 2)

                def qk(kt, S, sres, h=h, kb=kb):
                    mm(S, kth[kb][0:96, kt * 128:(kt + 1) * 128], qm[0:96, h, :], True, True, [("kth", kb), "qm"], [sres])

                def pv(kt, O, Pt, st, sp_, pres, ores, kb=kb):
                    mm(O, vah[kb][:, kt, :], Pt, st, sp_, [("vah", kb), pres], [ores])

                ob = attend(list(range(NKT)), qk, pv, [None] * NKT, 96 ** -0.5)
                finish(ob, oT[:, h, :])
            for g in range(2):
                for qbk in range(4):
                    blk = ch * 4 + qbk
                    tiles, masks = [0, 1], [None, None]
                    for kbk, mk in ((blk - 1, 0), (blk, None), (blk + 1, 128)):
                        if 0 <= kbk < 32:
                            tiles.append(2 + kbk)
                            masks.append(mk)
                    gs_ = slice(g * 64, (g + 1) * 64)

                    def qk(kt, S, sres, gs_=gs_, qbk=qbk):
                        mm(S, kst[gs_, kt * 128:(kt + 1) * 128], qs[gs_, qbk, :], True, True, ["kst", "qs"], [sres])

                    def pv(kt, O, Pt, st, sp_, pres, ores, g=g):
                        mm(O, vsa[:, kt, g * 65:(g + 1) * 65], Pt, st, sp_, ["vsa", pres], [ores])

                    ob = attend(tiles, qk, pv, masks, 0.125)
                    finish(ob, oT[:, 8 + g * 4:8 + g * 4 + 4, qbk * 128:(qbk + 1) * 128], add_sink=esk[64:65, g * 512:(g + 1) * 512])
            dma("sp", s_o[:, :, tok0:tok0 + 512], oT[:], ["oT"], [], 3)
        P.barrier()
        A.reset(markB)

    if stage >= 3:
        Wa = A([64, 8, D], BF16, "Wa")
        Wb = A([64, 8, D], BF16, "Wb")
        Wo = A([128, 8, D], BF16, "Wo")
        wr = A([128, 8, NE], F32, "wr")
        dma("pool", Wa[:], wba_d.rearrange("(h p) n -> p h n", p=64), [], ["Wa"], 1)
        dma("pool", Wb[:], wbb_d.rearrange("(h p) n -> p h n", p=64), [], ["Wb"], 1)
        dma("pool", Wo[:], wout_d.rearrange("(k p) n -> p k n", p=128), [], ["Wo"], 1)
        dma("sp", wr[:], wr_d.rearrange("(k p) n -> p k n", p=128), [], ["wr"], 2)
        oc = A([64, 16, 512], BF16)
        sgc = A([128, 16, 512], BF16)
        t1 = A([128, 512], F32)
        t2 = A([128, 512], F32)
        yT = A([128, 8, 512], BF16)
        xt = [A([128, D], F32) for _ in range(2)]
        xnew = [A([128, D], F32) for _ in range(2)]
        tmp = A([128, D], F32)
        junk = A([128, D], F32)
        xn2 = A([128, D], F32)
        ssq = A([128, 1], F32)
        rstd = A([128, 1], F32)
        tmpf = A([128, 8, 128], F32)
        h2f = A([128, 8, 128], F32)
        h2b = [A([128, 8, 128], BF16) for _ in range(2)]
        lg = A([128, NE], F32)
        mx8 = A([128, 8], F32)
        nmx = A([128, 1], F32)
        msk = A([128, NE], F32)
        ee = A([128, NE], F32)
        esum = A([128, 1], F32)
        wts = A([128, NE], F32)
        T32 = PB[:, 5:7, :].rearrange("p a (k t) -> p (a k) t", t=128)
        tctr = 0
        for ch in range(8):
            tok0 = ch * 512
            dma("sp", oc[:], s_o[:, :, tok0:tok0 + 512], [], ["oc"], 2)
            dma("sp", sgc[:], s_gate[:, :, tok0:tok0 + 512], [], ["sgc"], 2)
            for c in range(8):
                for h in range(8):
                    mm(PB[:, 0, :], Wa[:, h, c * 128:(c + 1) * 128], oc[:, h, :], h == 0, h == 7, ["Wa", "oc"], [("pb", 0)])
                for h in range(8):
                    mm(PB[:, 1, :], Wb[:, h, c * 128:(c + 1) * 128], oc[:, 8 + h, :], h == 0, h == 7, ["Wb", "oc"], [("pb", 1)])
                tt("dve", t1[:], PB[:, 0, :], sgc[:, c, :], ALU.mult, [("pb", 0), "sgc"], ["t1"])
                tt("dve", t2[:], PB[:, 1, :], sgc[:, 8 + c, :], ALU.mult, [("pb", 1), "sgc"], ["t2"])
                tt("pool", yT[:, c, :], t1[:], t2[:], ALU.add, ["t1", "t2"], [("yT", c)])
            YT = [("yT", c) for c in range(8)]
            for t in range(4):
                b = tctr % 2
                tctr += 1
                r0 = tok0 + t * 128
                dma("sp", xt[b][:], x_d[r0:r0 + 128, :], [], [("xt", b)], 4)
                for half in range(2):
                    hs = slice(half * 512, (half + 1) * 512)
                    bk = 2 + half
                    for c in range(8):
                        mm(PB[:, bk, :], yT[:, c, t * 128:(t + 1) * 128], Wo[:, c, hs], c == 0, c == 7, YT + ["Wo"], [("pb", bk)])
                    tt("dve", tmp[:, hs], PB[:, bk, :], g1bc[:, hs], ALU.mult, [("pb", bk), ("g1bc", half)], [("tmp", half)])
                    tt("pool", xnew[b][:, hs], tmp[:, hs], xt[b][:, hs], ALU.add, [("tmp", half), ("xt", b)], [("xnew", b, half)])
                XN = [("xnew", b, 0), ("xnew", b, 1)]
                dma("sp", s_xnew[r0:r0 + 128, :], xnew[b][:], XN, [], 3)
                act(junk[:], xnew[b][:], AF.Square, XN, ["junk", "ssq"], accum_out=ssq[:])
                rsqrt(ssq[:], rstd[:], 1024.0, ["ssq"], ["rstd"], "x2")
                act(xn2[:], xnew[b][:], AF.Copy, XN + ["rstd"], ["xn2"], scale=rstd[:, 0:1])
                for k in range(8):
                    tr(T32[:, k, :], xn2[:, k * 128:(k + 1) * 128], identf[:], ["xn2", "identf"], ["T32"])
                tt("dve", tmpf[:], T32, bcast(gs2[:], 2, [128, 8, 128]), ALU.mult, ["T32", "gs2"], ["tmpf"])
                tt("dve", h2f[:], tmpf[:], bcast(sh2[:], 2, [128, 8, 128]), ALU.add, ["tmpf", "sh2"], ["h2f"])
                cp("pool", h2b[b][:], h2f[:], ["h2f"], [("h2b", b)])
                dma("sp", s_h2[:, :, r0:r0 + 128], h2b[b][:], [("h2b", b)], [], 3)
                for k in range(8):
                    mm(PB[:, 4, 0:NE], h2f[:, k, :], wr[:, k, :], k == 0, k == 7, ["h2f", "wr"], [("pb", 4)])
                tt("dve", lg[:], PB[:, 4, 0:NE], rows_t[:, R_BR:R_BR + NE], ALU.add, [("pb", 4), "rows"], ["lg"])
                P.op("dve", lambda e: e.max(out=mx8[:], in_=lg[:]), reads=["lg"], writes=["mx8"])
                ts("dve", msk[:], lg[:], mx8[:, 3:4], None, ALU.is_ge, None, ["lg", "mx8"], ["msk"])
                ts("dve", nmx[:], mx8[:, 0:1], -1.0, None, ALU.mult, None, ["mx8"], ["nmx"])
                act(ee[:], lg[:], AF.Exp, ["lg", "nmx"], ["ee"], bias=nmx[:, 0:1], scale=1.0)
                tt("dve", ee[:], ee[:], msk[:], ALU.mult, ["ee", "msk"], ["ee"])
                P.op("dve", lambda e: e.reduce_sum(out=esum[:], in_=ee[:], axis=AX.X), reads=["ee"], writes=["esum"])
                P.op("dve", lambda e: e.reciprocal(out=esum[:], in_=esum[:]), reads=["esum"], writes=["esum"])
                ts("dve", wts[:], ee[:], esum[:, 0:1], None, ALU.mult, None, ["ee", "esum"], ["wts"])
                cp("pool", Wtok[:, r0 // 128, :], wts[:], ["wts"], [("Wtok", r0 // 128)])
                tr(PB[0:32, 4, 256:384], wts[:], identf[:], ["wts", "identf"], [("pb", 4)])
                cp("act", WT[:, r0:r0 + 128], PB[0:32, 4, 256:384], [("pb", 4)], [("WT", r0)])
                cp("dve", WTb[:, r0:r0 + 128], PB[0:32, 4, 256:384], [("pb", 4)], [("WTb", r0)])
        if dbg:
            dma("sp", s_wt, WT[:], [("WT", r) for r in range(0, NTOK, 128)], [], 3)
        P.barrier()
        A.reset(markA)

    if stage >= 4:
        wgu = [A([128, 8, 2 * D], BF16) for _ in range(2)]
        wdn = A([128, 8, D], BF16)
        bdn = A([32, D], BF16)
        h2c = A([128, 8, 1024], BF16)
        yacc = A([128, 8, D], F32)
        actT = [A([128, 8, 512], BF16) for _ in range(2)]
        gl = A([128, 512], F32)
        sgm = A([128, 512], F32)
        lb = A([128, 512], F32)
        tA = A([128, 512], F32)
        xo = [A([128, D], F32) for _ in range(1)]
        fo = [A([128, D], F32) for _ in range(2)]
        dma("pool", bdn[:], bdn_d, [], ["bdn"], 1)
        units = [(tc, e, sub) for tc in range(4) for e in range(NE) for sub in range(2)]
        wgu_v = wgu_d.rearrange("e (k p) n -> e p k n", p=128)
        wdn_v = wdn_d.rearrange("e (k p) n -> e p k n", p=128)
        gctr = [0]

        def load_w(tc, e):
            b = e % 2
            for k in range(8):
                for c2 in range(2):
                    dma("pool", wgu[b][:, k, c2 * D:(c2 + 1) * D], wgu_v[e, :, k, c2 * D:(c2 + 1) * D], [], [("wgu", b, k)], 5 if b == 0 else 8)

        def load_dn(e):
            for k2 in range(4):
                dma("pool", wdn[:, 2 * k2:2 * k2 + 2, :], wdn_v[e, :, 2 * k2:2 * k2 + 2, :], [], [("wdn", k2)], 6)

        def GU(ui):
            tc, e, sub = units[ui]
            b = e % 2
            ab = ui % 2
            t0 = tc * 1024 + sub * 512
            hs = slice(sub * 512, (sub + 1) * 512)
            for j in range(8):
                gb = gctr[0] % 2
                gctr[0] += 1
                for k in range(8):
                    mm(PB[:, gb, :], wgu[b][:, k, j * 128:(j + 1) * 128], h2c[:, k, hs], k == 0, k == 7, [("wgu", b, k), "h2c"], [("pb", gb)])
                for k in range(8):
                    mm(PB[:, 2 + gb, :], wgu[b][:, k, D + j * 128:D + (j + 1) * 128], h2c[:, k, hs], k == 0, k == 7,
                       [("wgu", b, k), "h2c"], [("pb", 2 + gb)])
                cg = C_BGU + e * 16 + j
                ts("dve", gl[:], PB[:, gb, :], cols_t[:, cg:cg + 1], 7.0, ALU.add, ALU.min, [("pb", gb), "cols"], ["gl"])
                act(sgm[:], gl[:], AF.Sigmoid, ["gl"], ["sgm"], scale=1.702)
                act(lb[:], PB[:, 2 + gb, :], AF.Identity, [("pb", 2 + gb), "cols"], ["lb"], bias=cols_t[:, cg + 8:cg + 9], scale=1.0)
                ts("dve", lb[:], lb[:], -7.0, 7.0, ALU.max, ALU.min, ["lb"], ["lb"])
                tt("dve", tA[:], gl[:], sgm[:], ALU.mult, ["gl", "sgm"], ["tA"])
                stt("dve", actT[ab][:, j, :], lb[:], 1.0, tA[:], ALU.add, ALU.mult, ["lb", "tA"], [("actT", ab, j)])

        def DN(ui):
            tc, e, sub = units[ui]
            ab = ui % 2
            AT = [("actT", ab, j) for j in range(8)]
            for t in range(4):
                tg = sub * 4 + t
                r0 = tc * 1024 + tg * 128
                for half in range(2):
                    hs = slice(half * 512, (half + 1) * 512)
                    bk = 4 + half
                    if e == 0:
                        mm(PB[:, bk, :], WTb[:, r0:r0 + 128], bdn[:, hs], True, True, [("WTb", r0), "bdn"], [("pb", bk)])
                        cp("act", yacc[:, tg, hs], PB[:, bk, :], [("pb", bk)], [("yacc", tg, half)])
                    for j in range(8):
                        mm(PB[:, bk, :], actT[ab][:, j, t * 128:(t + 1) * 128], wdn[:, j, hs], j == 0, j == 7,
                           AT + [("wdn", j // 2)], [("pb", bk)])
                    stt("dve", yacc[:, tg, hs], PB[:, bk, :], Wtok[:, r0 // 128, e:e + 1], yacc[:, tg, hs], ALU.mult, ALU.add,
                        [("pb", bk), ("yacc", tg, half), ("Wtok", r0 // 128)], [("yacc", tg, half)])

        def FIN(tc):
            for tg in range(8):
                b = tg % 2
                r0 = tc * 1024 + tg * 128
                dma("sp", xo[0][:], s_xnew[r0:r0 + 128, :], [], [("xo", 0)], 2)
                tt("dve", fo[b][:], yacc[:, tg, :], g2bc[:], ALU.mult, [("yacc", tg, 0), ("yacc", tg, 1), ("g2bc", 0), ("g2bc", 1)], [("fo", b)])
                tt("dve", fo[b][:], fo[b][:], xo[0][:], ALU.add, [("fo", b), ("xo", 0)], [("fo", b)])
                dma("sp", out_d[r0:r0 + 128, :], fo[b][:], [("fo", b)], [], 7)

        nU = len(units)
        for ui in range(nU + 1):
            if ui < nU:
                tc, e, sub = units[ui]
                if sub == 0:
                    if e == 0:
                        dma("sp", h2c[:], s_h2[:, :, tc * 1024:(tc + 1) * 1024], [], ["h2c"], 2)
                    if ui == 0:
                        load_w(tc, 0)
                    ne = ui + 2
                    if ne < nU:
                        load_w(units[ne][0], units[ne][1])
                    if ui == 0:
                        load_dn(0)
                GU(ui)
            if ui >= 1:
                DN(ui - 1)
                ptc, pe_, psub = units[ui - 1]
                if pe_ == NE - 1 and psub == 1:
                    FIN(ptc)
            if ui < nU and ui >= 1 and units[ui][2] == 0:
                load_dn(units[ui][1])
    P.emit()
    return nc, P


def _rope_tables(dim):
    q = dim // 4
    n = NTOK
    row = np.repeat(np.arange(n // 64, dtype=np.int32), 64).astype(np.float32)
    col = np.tile(np.arange(64, dtype=np.int32), n // 64).astype(np.float32)
    freqs = (np.float32(10000.0) ** (-np.arange(q, dtype=np.float32) / np.float32(q))).astype(np.float32)
    ar = row[:, None] * freqs
    ac = col[:, None] * freqs
    ang = np.concatenate([ar, ar, ac, ac], axis=-1).astype(np.float32)
    cos = np.cos(ang).astype(np.float32)
    sin = np.sin(ang).astype(np.float32)
    sgn = np.concatenate([-np.ones(q), np.ones(q), -np.ones(q), np.ones(q)]).astype(np.float32)
    return cos, sin * sgn


def _prep(inp):
    f = lambda a: np.ascontiguousarray(np.asarray(a, dtype=np.float32))
    L = 0
    cm, sm = _rope_tables(32)
    cs, ss = _rope_tables(64)
    rope = f(np.concatenate([cm, sm, cs, ss], axis=1).reshape(32, 128, 192))
    kj = np.arange(128)[:, None]
    qi = np.arange(128)[None, :]
    masks = f(np.concatenate([(qi <= kj), (qi >= kj)], axis=1))
    bgu = np.asarray(inp["b_gate_up"][L], np.float32)
    bgu_cols = np.zeros((128, 512), np.float32)
    for e in range(NE):
        bgu_cols[:, e * 16:e * 16 + 8] = bgu[e, 0::2].reshape(8, 128).T
        bgu_cols[:, e * 16 + 8:e * 16 + 16] = bgu[e, 1::2].reshape(8, 128).T
    wgu = np.asarray(inp["w_gate_up"][L], np.float32)
    wgu_p = f(np.concatenate([wgu[:, :, 0::2], wgu[:, :, 1::2]], axis=2))
    shared = dict(
        rope=rope, masks=masks,
        w_ada=f(inp["w_ada"][L]), w_in=f(inp["w_in"][L]), w_q_up=f(inp["w_q_up"][L]), w_kv_up=f(inp["w_kv_up"][L]),
        w_branch_a=f(inp["w_branch_a"][L]), w_branch_b=f(inp["w_branch_b"][L]), w_out=f(inp["w_out"][L]),
        w_router=f(inp["w_router"][L]), w_gu=wgu_p, w_dn=f(inp["w_down"][L]), b_dn=f(inp["b_down"][L]),
    )
    b_ada = np.asarray(inp["b_ada"][L], np.float32)
    rep = lambda v: np.broadcast_to(np.asarray(v, np.float32)[None, :], (128, len(v)))
    rows = f(np.concatenate([
        rep(inp["mla_q_g"][L]), rep(inp["mla_k_g"][L]), rep(inp["swa_q_g"][L]), rep(inp["swa_k_g"][L]),
        rep(inp["b_router"][L]), rep(np.repeat(np.asarray(inp["swa_sink"][L], np.float32), 128)),
        rep(b_ada[2048:3072]), rep(b_ada[5120:6144])], axis=1))
    assert rows.shape == (128, NROW)
    in_maps = []
    for b in range(8):
        colT = lambda v: np.asarray(v, np.float32).reshape(-1, 128).T
        cols = f(np.concatenate([
            colT(b_ada), colT(inp["norm1_g"][L]), colT(inp["norm2_g"][L]), colT(inp["c"][b]), colT(inp["c_ctx"]),
            colT(inp["mla_q_a_g"][L]), colT(inp["mla_kv_a_g"][L]), bgu_cols], axis=1))
        assert cols.shape == (128, NCOL)
        m = dict(shared)
        m.update(x=f(inp["x"][b]), ctx=f(inp["ctx"][b]), cols=cols, rows=rows)
        in_maps.append(m)
    return in_maps


_CACHE = {}


def kernel(**inputs):
    in_maps = _prep(inputs)
    if "nc" not in _CACHE:
        _CACHE["nc"] = build()[0]
    res = run_bass_kernel_spmd(_CACHE["nc"], in_maps, core_ids=list(range(8)))
    return np.stack([np.asarray(r["out"], dtype=np.float32) for r in res.results], axis=0)
```
